# Optimizing a Trainium2 kernel written in Bass

```python
import math
import jax, jax.numpy as jnp
from jax import lax
import numpy as np

D_MODEL = 2048
BATCH = 4
SEQ = 4096
DEPTH = 2

GRID_W = 64
CTX_LEN = 256
HEAD_DIM = 128
N_GROUP_HEADS = D_MODEL // (2 * HEAD_DIM)
MIX_WIDTH = 2 * N_GROUP_HEADS * HEAD_DIM
Q_BLOCK = 128
ROPE_BASE = 10000.0
EPS = 1e-6
NEG_INF = -1e30
A_HEADS = N_GROUP_HEADS
A_SUB = HEAD_DIM // 2
A_SCALE = A_SUB ** -0.5
LAMBDA_INIT_L0 = 0.8 - 0.6 * math.exp(-0.3 * 0)
B_HEADS = N_GROUP_HEADS
B_KV = B_HEADS // 4
B_GROUP = B_HEADS // B_KV
C_HEADS = N_GROUP_HEADS
C_KV = C_HEADS // 4
C_GROUP = C_HEADS // C_KV
C_WINDOW = 128
D_HEADS = N_GROUP_HEADS
NA_ROWS = 8
NA_COLS = 16
NA_QCOLS = 16
NA_KCOLS = NA_COLS + NA_QCOLS
ATT_SCALE = HEAD_DIM ** -0.5
D_FF = 5632
N_EXPERTS = 8
TOP_K = 2
D_EXPERT = 7168
MOE_BLOCK = 128

L0_SIZES = (A_HEADS * HEAD_DIM, B_HEADS * HEAD_DIM, A_HEADS * HEAD_DIM, A_HEADS * HEAD_DIM, B_KV * HEAD_DIM, B_KV * HEAD_DIM)
L1_SIZES = (C_HEADS * HEAD_DIM, D_HEADS * HEAD_DIM, C_KV * HEAD_DIM, C_KV * HEAD_DIM, D_HEADS * HEAD_DIM, D_HEADS * HEAD_DIM)
L0_Q = L0_SIZES[0] + L0_SIZES[1]
L1_Q = L1_SIZES[0] + L1_SIZES[1]
PROJ_COLS = sum(L0_SIZES)

kernel_name = 'hybrid_diffusion_prefix_block'


def _split(t, sizes):
    return jnp.split(t, [int(i) for i in np.cumsum(sizes)[:-1]], axis=-1)


def rmsnorm(x, g):
    xf = x.astype(jnp.float32)
    y = xf * lax.rsqrt(jnp.mean(xf * xf, axis=-1, keepdims=True) + EPS)
    return (y * g.astype(jnp.float32)).astype(x.dtype)


def modulate(x, g, shift, scale):
    return rmsnorm(x, g) * (1.0 + scale) + shift


def ada_mod(c_vec, w_ada, b_ada, n_chunks):
    m = jax.nn.silu(c_vec) @ w_ada[:, :n_chunks * D_MODEL] + b_ada[:n_chunks * D_MODEL]
    return jnp.split(m, n_chunks, axis=-1)


def axial_rope(n_tok, dim):
    t = jnp.arange(n_tok)
    pos = jnp.stack([t // GRID_W, t % GRID_W], axis=-1).astype(jnp.float32)
    n_pairs = dim // 4
    freq = ROPE_BASE ** (-jnp.arange(n_pairs, dtype=jnp.float32) / n_pairs)
    ang = (pos[:, :, None] * freq).reshape(n_tok, 2 * n_pairs)
    return jnp.cos(ang), jnp.sin(ang)


def apply_rope(x, cos, sin):
    xf = x.astype(jnp.float32).reshape(x.shape[:-1] + (-1, 2))
    x0, x1 = xf[..., 0], xf[..., 1]
    shape = (1, cos.shape[0]) + (1,) * (x.ndim - 3) + (cos.shape[1],)
    c, s = cos.reshape(shape), sin.reshape(shape)
    out = jnp.stack([x0 * c - x1 * s, x0 * s + x1 * c], axis=-1)
    return out.reshape(x.shape).astype(x.dtype)


def gqa_attend(q, k, v, sink=None):
    s = jnp.einsum('bqhgd,bkhd->bhgqk', q, k).astype(jnp.float32) * ATT_SCALE
    if sink is None:
        p = jax.nn.softmax(s, axis=-1)
    else:
        sk = jnp.broadcast_to(sink.astype(jnp.float32).reshape(1, k.shape[2], -1, 1, 1), s.shape[:-1] + (1,))
        p = jax.nn.softmax(jnp.concatenate([s, sk], axis=-1), axis=-1)[..., :-1]
    o = jnp.einsum('bhgqk,bkhd->bqhgd', p.astype(v.dtype), v)
    return o.reshape(q.shape[:2] + (-1,))


def diff_attend(q, k, v, lam, subln_g):
    s = jnp.einsum('bqhcd,bkhcd->bhcqk', q, k).astype(jnp.float32) * A_SCALE
    p = jax.nn.softmax(s, axis=-1)
    a = p[:, :, 0] - lam * p[:, :, 1]
    o = jnp.einsum('bhqk,bkhd->bqhd', a.astype(v.dtype), v)
    o = rmsnorm(o, subln_g) * (1.0 - LAMBDA_INIT_L0)
    return o.reshape(q.shape[:2] + (-1,))


def sweep_query_blocks(fn, *qs):
    b, s = qs[0].shape[:2]
    nb = s // Q_BLOCK
    blocks = tuple(jnp.moveaxis(q.reshape((b, nb, Q_BLOCK) + q.shape[2:]), 1, 0) for q in qs)
    out = lax.map(lambda a: fn(*a), blocks)
    return jnp.moveaxis(out, 0, 1).reshape((b, s) + out.shape[3:])


def window_attend(q, k, v, kc, vc, sinks):
    b, s, hkv, g, d = q.shape
    nb = s // Q_BLOCK
    ns = C_WINDOW // Q_BLOCK
    band = (2 * ns + 1) * Q_BLOCK
    pad = ((0, 0), (C_WINDOW, C_WINDOW), (0, 0), (0, 0))
    kb = jnp.pad(k, pad).reshape(b, nb + 2 * ns, Q_BLOCK, hkv, d)
    vb = jnp.pad(v, pad).reshape(b, nb + 2 * ns, Q_BLOCK, hkv, d)
    k_band = jnp.concatenate([kb[:, j:j + nb] for j in range(2 * ns + 1)], axis=2)
    v_band = jnp.concatenate([vb[:, j:j + nb] for j in range(2 * ns + 1)], axis=2)
    qb = q.reshape(b, nb, Q_BLOCK, hkv, g, d)
    qpos = jnp.arange(s).reshape(nb, Q_BLOCK)
    kpos = jnp.arange(nb)[:, None] * Q_BLOCK - C_WINDOW + jnp.arange(band)[None, :]
    ok = (jnp.abs(kpos[:, None, :] - qpos[:, :, None]) <= C_WINDOW) & (kpos[:, None, :] >= 0) & (kpos[:, None, :] < s)
    s_loc = jnp.einsum('bnqhgd,bnkhd->bhgnqk', qb, k_band).astype(jnp.float32) * ATT_SCALE
    s_loc = jnp.where(ok, s_loc, NEG_INF)
    s_ctx = jnp.einsum('bnqhgd,bkhd->bhgnqk', qb, kc).astype(jnp.float32) * ATT_SCALE
    sk = jnp.broadcast_to(sinks.astype(jnp.float32).reshape(1, hkv, g, 1, 1, 1), s_loc.shape[:-1] + (1,))
    p = jax.nn.softmax(jnp.concatenate([s_loc, s_ctx, sk], axis=-1), axis=-1).astype(v.dtype)
    o = (jnp.einsum('bhgnqk,bnkhd->bnqhgd', p[..., :band], v_band)
         + jnp.einsum('bhgnqk,bkhd->bnqhgd', p[..., band:-1], vc))
    return o.reshape(b, s, hkv * g * d)


def neighbourhood_attend(q, k, v, kc, vc, rpb):
    b, s, h, d = q.shape
    rows = s // GRID_W
    wr = min(NA_ROWS, rows)
    n_cb = GRID_W // NA_QCOLS
    cb = np.clip(np.arange(n_cb) * NA_QCOLS - NA_COLS // 2, 0, GRID_W - NA_KCOLS)
    kcol = cb[:, None] + np.arange(NA_KCOLS)[None, :]
    qcol = np.arange(GRID_W).reshape(n_cb, NA_QCOLS)
    cstart = np.clip(qcol - NA_COLS // 2, 0, GRID_W - NA_COLS)
    col_ok = (kcol[:, None, :] >= cstart[:, :, None]) & (kcol[:, None, :] < cstart[:, :, None] + NA_COLS)
    dcol = np.clip(kcol[:, None, :] - qcol[:, :, None] + NA_COLS - 1, 0, 2 * NA_COLS - 2)
    kg = k.reshape(b, rows, GRID_W, h, d)
    vg = v.reshape(b, rows, GRID_W, h, d)
    qg = jnp.moveaxis(q.reshape(b, rows, GRID_W, h, d), 1, 0)

    def row_block(args):
        r, q_row = args
        rs = jnp.clip(r - wr // 2, 0, rows - wr)
        k_nb = lax.dynamic_slice_in_dim(kg, rs, wr, axis=1)[:, :, kcol]
        v_nb = lax.dynamic_slice_in_dim(vg, rs, wr, axis=1)[:, :, kcol]
        qb = q_row.reshape(b, n_cb, NA_QCOLS, h, d)
        drow = rs + jnp.arange(wr) - r + NA_ROWS - 1
        bias = rpb[:, drow[None, None, :, None], dcol[:, :, None, :]].astype(jnp.float32)
        s_nb = jnp.einsum('bcqhd,bwckhd->bhcqwk', qb, k_nb).astype(jnp.float32) * ATT_SCALE + bias
        s_nb = jnp.where(col_ok[:, :, None, :], s_nb, NEG_INF).reshape(b, h, n_cb, NA_QCOLS, wr * NA_KCOLS)
        s_ctx = jnp.einsum('bcqhd,bkhd->bhcqk', qb, kc).astype(jnp.float32) * ATT_SCALE
        p = jax.nn.softmax(jnp.concatenate([s_nb, s_ctx], axis=-1), axis=-1).astype(v.dtype)
        p_nb = p[..., :wr * NA_KCOLS].reshape(b, h, n_cb, NA_QCOLS, wr, NA_KCOLS)
        o = (jnp.einsum('bhcqwk,bwckhd->bcqhd', p_nb, v_nb)
             + jnp.einsum('bhcqk,bkhd->bcqhd', p[..., wr * NA_KCOLS:], vc))
        return o.reshape(b, GRID_W, h * d)

    o = lax.map(row_block, (jnp.arange(rows), qg))
    return jnp.moveaxis(o, 0, 1).reshape(b, s, h * d)


def mixer_diff_gqa(h, hc, w_in, lam_q1, lam_k1, lam_q2, lam_k2, subln_g, q_norm_g, k_norm_g, rope, ctx_out):
    cos_a, sin_a, cos_b, sin_b = rope
    b, s, _ = h.shape
    n_ctx = hc.shape[1]
    aq, bq, ak, av, bk, bv = _split(h @ w_in, L0_SIZES)
    aq = apply_rope(aq.reshape(b, s, A_HEADS, 2, A_SUB), cos_a, sin_a)
    ak = apply_rope(ak.reshape(b, s, A_HEADS, 2, A_SUB), cos_a, sin_a)
    bq = apply_rope(rmsnorm(bq.reshape(b, s, B_KV, B_GROUP, HEAD_DIM), q_norm_g), cos_b, sin_b)
    bk = apply_rope(rmsnorm(bk.reshape(b, s, B_KV, HEAD_DIM), k_norm_g), cos_b, sin_b)
    if ctx_out:
        aqc, bqc, akc, avc, bkc, bvc = _split(hc @ w_in, L0_SIZES)
    else:
        akc, avc, bkc, bvc = _split(hc @ w_in[:, L0_Q:], L0_SIZES[2:])
    akc = akc.reshape(b, n_ctx, A_HEADS, 2, A_SUB)
    avc = avc.reshape(b, n_ctx, A_HEADS, HEAD_DIM)
    bkc = rmsnorm(bkc.reshape(b, n_ctx, B_KV, HEAD_DIM), k_norm_g)
    bvc = bvc.reshape(b, n_ctx, B_KV, HEAD_DIM)
    lam = (jnp.exp(jnp.sum((lam_q1 * lam_k1).astype(jnp.float32)))
           - jnp.exp(jnp.sum((lam_q2 * lam_k2).astype(jnp.float32))) + LAMBDA_INIT_L0)
    ak_all = jnp.concatenate([ak, akc], axis=1)
    av_all = jnp.concatenate([av.reshape(b, s, A_HEADS, HEAD_DIM), avc], axis=1)
    bk_all = jnp.concatenate([bk, bkc], axis=1)
    bv_all = jnp.concatenate([bv.reshape(b, s, B_KV, HEAD_DIM), bvc], axis=1)

    def block(aq_blk, bq_blk):
        return jnp.concatenate([diff_attend(aq_blk, ak_all, av_all, lam, subln_g),
                                gqa_attend(bq_blk, bk_all, bv_all)], axis=-1)

    o_lat = sweep_query_blocks(block, aq, bq)
    if not ctx_out:
        return o_lat, None
    aqc = aqc.reshape(b, n_ctx, A_HEADS, 2, A_SUB)
    bqc = rmsnorm(bqc.reshape(b, n_ctx, B_KV, B_GROUP, HEAD_DIM), q_norm_g)
    o_ctx = jnp.concatenate([diff_attend(aqc, akc, avc, lam, subln_g), gqa_attend(bqc, bkc, bvc)], axis=-1)
    return o_lat, o_ctx


def mixer_window_na(h, hc, w_in, sinks, rpb, rope, ctx_out):
    _, _, cos_b, sin_b = rope
    b, s, _ = h.shape
    n_ctx = hc.shape[1]
    cq, dq, ck, cv, dk, dv = _split(h @ w_in, L1_SIZES)
    cq = apply_rope(cq.reshape(b, s, C_KV, C_GROUP, HEAD_DIM), cos_b, sin_b)
    ck = apply_rope(ck.reshape(b, s, C_KV, HEAD_DIM), cos_b, sin_b)
    cv = cv.reshape(b, s, C_KV, HEAD_DIM)
    dq = dq.reshape(b, s, D_HEADS, HEAD_DIM)
    dk = dk.reshape(b, s, D_HEADS, HEAD_DIM)
    dv = dv.reshape(b, s, D_HEADS, HEAD_DIM)
    if ctx_out:
        cqc, dqc, ckc, cvc, dkc, dvc = _split(hc @ w_in, L1_SIZES)
    else:
        ckc, cvc, dkc, dvc = _split(hc @ w_in[:, L1_Q:], L1_SIZES[2:])
    ckc = ckc.reshape(b, n_ctx, C_KV, HEAD_DIM)
    cvc = cvc.reshape(b, n_ctx, C_KV, HEAD_DIM)
    dkc = dkc.reshape(b, n_ctx, D_HEADS, HEAD_DIM)
    dvc = dvc.reshape(b, n_ctx, D_HEADS, HEAD_DIM)
    o_lat = jnp.concatenate([window_attend(cq, ck, cv, ckc, cvc, sinks),
                             neighbourhood_attend(dq, dk, dv, dkc, dvc, rpb)], axis=-1)
    if not ctx_out:
        return o_lat, None
    o_ctx = jnp.concatenate([gqa_attend(cqc.reshape(b, n_ctx, C_KV, C_GROUP, HEAD_DIM), ckc, cvc, sinks),
                             gqa_attend(dqc.reshape(b, n_ctx, D_HEADS, 1, HEAD_DIM), dkc, dvc)], axis=-1)
    return o_lat, o_ctx


def dense_swiglu(h, w_gate, w_up, w_down):
    return (jax.nn.silu(h @ w_gate) * (h @ w_up)) @ w_down


def moe_swiglu(h, w_router, w_gate_e, w_up_e, w_down_e):
    b, s, d = h.shape
    xt = h.reshape(-1, d)
    n = xt.shape[0]
    logits = (xt @ w_router).astype(jnp.float32)
    top_logit, top_idx = lax.top_k(logits, TOP_K)
    gates = jax.nn.softmax(top_logit, axis=-1)
    e_flat = top_idx.reshape(-1)
    tok_flat = jnp.repeat(jnp.arange(n, dtype=jnp.int32), TOP_K)
    order = jnp.argsort(e_flat)
    e_sorted = e_flat[order]
    tok_sorted = tok_flat[order]
    g_sorted = gates.reshape(-1)[order]
    counts = jnp.bincount(e_flat, length=N_EXPERTS)
    padded = (counts + MOE_BLOCK - 1) // MOE_BLOCK * MOE_BLOCK
    pad_end = jnp.cumsum(padded)
    pad_start = pad_end - padded
    sort_start = jnp.cumsum(counts) - counts
    slot = pad_start[e_sorted] + (jnp.arange(n * TOP_K) - sort_start[e_sorted])
    n_blocks = (n * TOP_K + N_EXPERTS * (MOE_BLOCK - 1) + MOE_BLOCK - 1) // MOE_BLOCK
    cap = n_blocks * MOE_BLOCK
    slot_tok = jnp.full((cap,), n, dtype=jnp.int32).at[slot].set(tok_sorted)
    x_pad = jnp.concatenate([xt, jnp.zeros((1, d), xt.dtype)], axis=0)
    block_expert = jnp.minimum(jnp.searchsorted(pad_end, jnp.arange(n_blocks) * MOE_BLOCK, side='right'), N_EXPERTS - 1)

    def run_block(args):
        e, idx = args
        xb = x_pad[idx]
        return (jax.nn.silu(xb @ w_gate_e[e]) * (xb @ w_up_e[e])) @ w_down_e[e]

    y_pad = lax.map(run_block, (block_expert, slot_tok.reshape(n_blocks, MOE_BLOCK))).reshape(cap, d)
    y_sorted = y_pad[slot] * g_sorted[:, None].astype(y_pad.dtype)
    y = jax.ops.segment_sum(y_sorted, tok_sorted, num_segments=n)
    return y.reshape(b, s, d)


def hybrid_layer(x, xc, c, c_ctx, base, mixer_fn, mix_prm, ffn_fn, ffn_prm, rope, ctx_out):
    norm1_g, norm2_g, w_ada, b_ada, w_in, w_out = base
    sh1, sc1, gt1, sh2, sc2, gt2 = [m[:, None] for m in ada_mod(c, w_ada, b_ada, 6)]
    cm = ada_mod(c_ctx, w_ada, b_ada, 6 if ctx_out else 2)
    o_lat, o_ctx = mixer_fn(modulate(x, norm1_g, sh1, sc1), modulate(xc, norm1_g, cm[0], cm[1]), w_in,
                            *mix_prm, rope=rope, ctx_out=ctx_out)
    x = x + gt1 * (o_lat @ w_out)
    x = x + gt2 * ffn_fn(modulate(x, norm2_g, sh2, sc2), *ffn_prm)
    if not ctx_out:
        return x, None
    xc = xc + cm[2] * (o_ctx @ w_out)
    xc = xc + cm[5] * ffn_fn(modulate(xc, norm2_g, cm[3], cm[4]), *ffn_prm)
    return x, xc


def setup_inputs(seed: int = 0) -> dict:
    key = jax.random.key(seed)
    ks = iter(jax.random.split(key, 40))
    d = D_MODEL

    def nrm(shape, scale):
        return jax.random.normal(next(ks), shape, jnp.float32) * scale

    def gain(n):
        return 1.0 + nrm((n,), 0.1)

    inp = {}
    inp['x'] = nrm((BATCH, SEQ, d), 1.0)
    inp['c'] = nrm((BATCH, d), 1.0)
    inp['ctx'] = nrm((BATCH, CTX_LEN, d), 1.0)
    inp['c_ctx'] = nrm((d,), 1.0)
    inp['l0_norm1_g'] = gain(d)
    inp['l0_norm2_g'] = gain(d)
    inp['l0_w_ada'] = nrm((d, 6 * d), 0.5 * d ** -0.5)
    inp['l0_b_ada'] = nrm((6 * d,), 0.01)
    inp['l0_w_in'] = nrm((d, PROJ_COLS), d ** -0.5)
    inp['l0_w_out'] = nrm((MIX_WIDTH, d), MIX_WIDTH ** -0.5)
    inp['l0_lam_q1'] = nrm((A_SUB,), 0.1)
    inp['l0_lam_k1'] = nrm((A_SUB,), 0.1)
    inp['l0_lam_q2'] = nrm((A_SUB,), 0.1)
    inp['l0_lam_k2'] = nrm((A_SUB,), 0.1)
    inp['l0_subln_g'] = gain(HEAD_DIM)
    inp['l0_q_norm_g'] = gain(HEAD_DIM)
    inp['l0_k_norm_g'] = gain(HEAD_DIM)
    inp['l0_ffn_w_gate'] = nrm((d, D_FF), d ** -0.5)
    inp['l0_ffn_w_up'] = nrm((d, D_FF), d ** -0.5)
    inp['l0_ffn_w_down'] = nrm((D_FF, d), D_FF ** -0.5)
    inp['l1_norm1_g'] = gain(d)
    inp['l1_norm2_g'] = gain(d)
    inp['l1_w_ada'] = nrm((d, 6 * d), 0.5 * d ** -0.5)
    inp['l1_b_ada'] = nrm((6 * d,), 0.01)
    inp['l1_w_in'] = nrm((d, PROJ_COLS), d ** -0.5)
    inp['l1_w_out'] = nrm((MIX_WIDTH, d), MIX_WIDTH ** -0.5)
    inp['l1_sinks'] = nrm((C_HEADS,), 0.5)
    inp['l1_rpb'] = nrm((D_HEADS, 2 * NA_ROWS - 1, 2 * NA_COLS - 1), 0.1)
    inp['l1_router'] = nrm((d, N_EXPERTS), d ** -0.5)
    inp['l1_exp_w_gate'] = nrm((N_EXPERTS, d, D_EXPERT), d ** -0.5)
    inp['l1_exp_w_up'] = nrm((N_EXPERTS, d, D_EXPERT), d ** -0.5)
    inp['l1_exp_w_down'] = nrm((N_EXPERTS, D_EXPERT, d), D_EXPERT ** -0.5)
    inp['final_norm_g'] = gain(d)
    return inp


def reference(x, c, ctx, c_ctx,
              l0_norm1_g, l0_norm2_g, l0_w_ada, l0_b_ada, l0_w_in, l0_w_out,
              l0_lam_q1, l0_lam_k1, l0_lam_q2, l0_lam_k2, l0_subln_g, l0_q_norm_g, l0_k_norm_g,
              l0_ffn_w_gate, l0_ffn_w_up, l0_ffn_w_down,
              l1_norm1_g, l1_norm2_g, l1_w_ada, l1_b_ada, l1_w_in, l1_w_out,
              l1_sinks, l1_rpb, l1_router, l1_exp_w_gate, l1_exp_w_up, l1_exp_w_down,
              final_norm_g):
    s = x.shape[1]
    rope = axial_rope(s, A_SUB) + axial_rope(s, HEAD_DIM)
    layers = [
        ((l0_norm1_g, l0_norm2_g, l0_w_ada, l0_b_ada, l0_w_in, l0_w_out), mixer_diff_gqa,
         (l0_lam_q1, l0_lam_k1, l0_lam_q2, l0_lam_k2, l0_subln_g, l0_q_norm_g, l0_k_norm_g),
         dense_swiglu, (l0_ffn_w_gate, l0_ffn_w_up, l0_ffn_w_down)),
        ((l1_norm1_g, l1_norm2_g, l1_w_ada, l1_b_ada, l1_w_in, l1_w_out), mixer_window_na,
         (l1_sinks, l1_rpb),
         moe_swiglu, (l1_router, l1_exp_w_gate, l1_exp_w_up, l1_exp_w_down)),
    ]
    xc = ctx
    for i in range(DEPTH):
        base, mixer_fn, mix_prm, ffn_fn, ffn_prm = layers[i]
        x, xc = hybrid_layer(x, xc, c, c_ctx, base, mixer_fn, mix_prm, ffn_fn, ffn_prm, rope,
                             ctx_out=(i < DEPTH - 1))
    return rmsnorm(x, final_norm_g)
```

```python
import math
import os
CUT = int(os.environ.get('KCUT', '99'))
from contextlib import ExitStack

import numpy as np
import concourse.bass as bass
import concourse.mybir as mybir
from concourse.bass_utils import run_bass_kernel_spmd

F32 = mybir.dt.float32
BF16 = mybir.dt.bfloat16
I32 = mybir.dt.int32
AF = mybir.ActivationFunctionType
ALU = mybir.AluOpType
AX = mybir.AxisListType

ENGS = ("tensor", "vector", "scalar", "gpsimd", "sync")
SEM_WRAP = 20000
N_DMA_SLOTS = 6

D = 2048
NK = 16
SEQ = 4096
NCTX = 256
NOWN = 2048
NHALO = 256
NQ0 = NOWN + NHALO + NCTX
NALL = SEQ + NCTX
EPS = 1e-6
LAMBDA_INIT_L0 = 0.8 - 0.6 * math.exp(-0.3 * 0)
D_FF = 5632
N_EXP = 8
D_EXP = 7168


class Buf:
    __slots__ = ("name", "last_w", "readers")

    def __init__(self, name=""):
        self.name = name
        self.last_w = None
        self.readers = []


class Op:
    __slots__ = ("eng", "fn", "waits", "is_dma", "slot", "use", "needs_inc", "clock", "semval")

    def __init__(self):
        self.needs_inc = False
        self.semval = None
        self.waits = []


class Prog:
    def __init__(self, nc, same_engine_sync=True):
        self.nc = nc
        self.ops = {e: [] for e in ENGS}
        self.known = {e: {} for e in ENGS}
        self.dma_slot_use = {}
        self.dma_slot_last = {}
        self.dma_rr = {e: 0 for e in ENGS}
        self.same_engine_sync = same_engine_sync
        self.last_ev = {e: None for e in ENGS}

    def _add(self, eng, fn, reads, writes, is_dma, extra_deps=()):
        op = Op()
        op.eng = eng
        op.fn = fn
        op.is_dma = is_dma
        deps = list(extra_deps)
        for b in reads:
            if b.last_w is not None:
                deps.append(b.last_w)
        for b in writes:
            if b.last_w is not None:
                deps.append(b.last_w)
            deps.extend(b.readers)
        if is_dma:
            slot = self.dma_rr[eng]
            self.dma_rr[eng] = (slot + 1) % N_DMA_SLOTS
            key = ("dma", eng, slot)
            use = self.dma_slot_use.get(key, 0) + 1
            self.dma_slot_use[key] = use
            prev = self.dma_slot_last.get(key)
            if prev is not None:
                deps.append(prev)
            op.slot = slot
            op.use = use
            ev = (key, use, op)
            self.dma_slot_last[key] = ev
        else:
            ev = (eng, len(self.ops[eng]), op)
        known = self.known[eng]
        changed = False
        best = {}
        for (k, i, dop) in deps:
            if known.get(k, -1) >= i:
                continue
            if k == eng and not is_dma:
                if eng == "tensor" or not self.same_engine_sync:
                    continue
            if not changed:
                known = dict(known)
                changed = True
            if k not in best or best[k][1] < i:
                best[k] = (k, i, dop)
            for kk, vv in dop.clock.items():
                if known.get(kk, -1) < vv:
                    known[kk] = vv
            if known.get(k, -1) < i:
                known[k] = i
        op.waits = list(best.values())
        for (k, i, dop) in op.waits:
            dop.needs_inc = True
        if changed:
            self.known[eng] = known
        if is_dma:
            c = dict(known)
            c[ev[0]] = ev[1]
            op.clock = c
        else:
            c = dict(known)
            c[eng] = ev[1]
            op.clock = c
            if fn is not None:
                self.last_ev[eng] = ev
        self.ops[eng].append(op)
        for b in reads:
            b.readers.append(ev)
        for b in writes:
            b.last_w = ev
            b.readers = []
        return ev

    def op(self, eng, fn, reads=(), writes=()):
        return self._add(eng, fn, reads, writes, False)

    def dma(self, eng, out, in_, reads=(), writes=(), **kw):
        return self._add(eng, lambda e: e.dma_start(out=out, in_=in_, **kw), reads, writes, True)

    def barrier(self):
        evs = [ev for ev in self.last_ev.values() if ev is not None] + list(self.dma_slot_last.values())
        for e in ENGS:
            if self.ops[e]:
                self._add(e, None, (), (), False, extra_deps=evs)

    def emit(self, final_events=()):
        nc = self.nc
        self._add("sync", None, (), (), False, extra_deps=list(final_events))
        n_sems = {}
        for e in ENGS:
            c = 0
            for o in self.ops[e]:
                if o.is_dma:
                    continue
                if o.needs_inc:
                    o.semval = (c // SEM_WRAP, c % SEM_WRAP + 1)
                    c += 1
            n_sems[e] = c // SEM_WRAP + 1
        with ExitStack() as st:
            sems = {}
            for e in ENGS:
                sems[e] = [st.enter_context(nc.semaphore(f"s_{e}_{i}")) for i in range(n_sems[e])]
            dsems = {}
            for key in self.dma_slot_use:
                dsems[key] = st.enter_context(nc.semaphore(f"d_{key[1]}_{key[2]}"))
            block = st.enter_context(nc.Block())

            def make(e):
                def body(eng):
                    pending_inc = None
                    for o in self.ops[e]:
                        for (k, i, dop) in o.waits:
                            if isinstance(k, tuple):
                                eng.wait_ge(dsems[k], 16 * i)
                            else:
                                si, v = dop.semval
                                eng.wait_ge(sems[k][si], v)
                        if o.fn is None:
                            assert not o.needs_inc
                            continue
                        inst = o.fn(eng)
                        if o.is_dma:
                            inst.then_inc(dsems[("dma", e, o.slot)], 16)
                        elif o.needs_inc:
                            inst.then_inc(sems[e][o.semval[0]], 1)
                return body

            for e in ENGS:
                if self.ops[e]:
                    getattr(block, e)(make(e))


def _rope_tables(pos_tok):
    n = len(pos_tok)
    out = np.zeros((n, 192), np.float32)
    valid = pos_tok >= 0
    t = np.where(valid, pos_tok, 0)
    pos = np.stack([t // 64, t % 64], axis=-1).astype(np.float32)
    col = 0
    for dim in (64, 128):
        n_pairs = dim // 4
        freq = (np.float32(10000.0) ** (-np.arange(n_pairs, dtype=np.float32) / np.float32(n_pairs))).astype(np.float32)
        ang = (pos[:, :, None] * freq).reshape(n, 2 * n_pairs).astype(np.float32)
        c = np.where(valid[:, None], np.cos(ang), 1.0).astype(np.float32)
        s = np.where(valid[:, None], np.sin(ang), 0.0).astype(np.float32)
        out[:, col:col + 2 * n_pairs] = c
        out[:, col + 2 * n_pairs:col + 4 * n_pairs] = s
        col += 4 * n_pairs
    return out


def _fm(v):
    return np.ascontiguousarray(v.reshape(-1, 128).T)


NEG = -30000.0


def _mask_c(half):
    m = np.zeros((8, 128, 512), np.float32)
    kj = np.arange(128)[:, None]
    qi = np.arange(128)[None, :]
    for r in range(-1, 5):
        for j in range(4):
            rel = r - j
            if rel == 0:
                blk = np.ones((128, 128), np.float32)
            elif rel == -1:
                blk = (kj >= qi).astype(np.float32)
            elif rel == 1:
                blk = (kj <= qi).astype(np.float32)
            else:
                continue
            m[r + 1, :, j * 128:(j + 1) * 128] = blk
    if half == 1:
        m[6] = m[0]
    else:
        m[7] = m[5]
    return m


def _na_bias_tile(rpb, drow):
    qc = np.arange(64)[None, :]
    kc = np.arange(64)[:, None]
    cstart = np.clip(qc - 8, 0, 48)
    ok = (kc >= cstart) & (kc < cstart + 16)
    dcol = np.clip(kc - qc + 15, 0, 30)
    if drow < 0 or drow > 14:
        return np.full((8, 64, 64), NEG, np.float32)
    b = rpb[:, drow][:, dcol]
    return np.where(ok[None], b, np.float32(NEG)).astype(np.float32)


def _bias_d(rpb):
    out = np.empty((8, 64, 8, 64), np.float32)
    for di in range(8):
        out[:, :, di, :] = _na_bias_tile(rpb, 3 + di)
    return out


def _bias_e(rpb, half):
    out = np.full((8, 8, 64, 12, 64), NEG, np.float32)
    for er in range(8):
        r = er if er < 4 else 24 + er
        R = r + 32 * half
        rs = min(max(R - 4, 0), 56)
        own = list(range(0, 8)) if er < 4 else list(range(24, 32))
        cand_global = [32 * half + o for o in own]
        halo_global = [32 + i for i in range(4)] if half == 0 else [28 + i for i in range(4)]
        for c, Rk in enumerate(cand_global + halo_global):
            if rs <= Rk < rs + 8:
                out[er, :, :, c, :] = _na_bias_tile(rpb, Rk - R + 7)
    return out


def core_token_order(half):
    own = np.arange(half * NOWN, (half + 1) * NOWN)
    halo = np.arange(NOWN, NOWN + NHALO) if half == 0 else np.arange(NOWN - NHALO, NOWN)
    mask = np.ones(SEQ, bool)
    mask[own] = False
    mask[halo] = False
    rest = np.nonzero(mask)[0]
    return np.concatenate([own, halo, rest])


class Ctx:
    pass


def build_program(upto=99, debug=False, start=0):
    nc = bass.Bass("TRN2", target_bir_lowering=False)
    P = Prog(nc)
    K = Ctx()
    K.nc, K.P = nc, P

    def din(name, shape, dt=F32):
        return nc.dram_tensor(name, list(shape), dt, kind="ExternalInput").ap()

    def dscr(name, shape, dt):
        kind = "ExternalOutput" if debug else "Internal"
        return nc.dram_tensor(name, list(shape), dt, kind=kind).ap()

    I = {}
    I["xT"] = din("xT", [D, NALL])
    I["cT"] = din("cT", [128, NK, 2])
    I["rope"] = din("rope", [NALL, 192])
    for l in (0, 1):
        I[f"w_ada{l}"] = din(f"w_ada{l}", [D, 6 * D])
        I[f"b_ada{l}"] = din(f"b_ada{l}", [128, 96])
        I[f"g1_{l}"] = din(f"g1_{l}", [128, NK])
        I[f"g2_{l}"] = din(f"g2_{l}", [128, NK])
        I[f"w_in{l}"] = din(f"w_in{l}", [D, 4608])
        I[f"w_out{l}"] = din(f"w_out{l}", [D, D])
    if start < 5:
        I["ffn_wg"] = din("ffn_wg", [D, D_FF])
        I["ffn_wu"] = din("ffn_wu", [D, D_FF])
        I["ffn_wd"] = din("ffn_wd", [D_FF, D])
    I["maskC"] = din("maskC", [8, 128, 512])
    I["sinks"] = din("sinks", [1, 8])
    I["biasD"] = din("biasD", [8, 64, 8, 64])
    I["biasE"] = din("biasE", [8, 8, 64, 12, 64])
    I["router"] = din("router", [D, N_EXP])
    if upto >= 8:
        I["exp_wg"] = din("exp_wg", [N_EXP, D, D_EXP])
        I["exp_wu"] = din("exp_wu", [N_EXP, D, D_EXP])
        I["exp_wd"] = din("exp_wd", [N_EXP, D_EXP, D])
    I["gfin"] = din("gfin", [128, NK])
    I["lam"] = din("lam", [1, 4, 64])
    I["hvec"] = din("hvec", [3, 128])
    K.I = I

    S = {}
    S["mod"] = dscr("s_mod", [2, 128, 6, NK, 2], F32)
    S["qT0"] = dscr("s_qT0", [16, 128, NQ0], BF16)
    S["kT0"] = dscr("s_kT0", [10, 128, NALL], BF16)
    S["v0"] = dscr("s_v0", [10, 128, NALL // 128, 128], BF16)
    S["att0"] = dscr("s_att0", [16, 128, NQ0], BF16)
    S["x1T"] = dscr("s_x1T", [D, NQ0], F32) if start < 5 else din("s_x1T", [D, NQ0])
    S["qT1"] = dscr("s_qT1", [16, 128, NOWN], BF16)
    S["kT1"] = dscr("s_kT1", [10, 128, NQ0], BF16)
    S["v1"] = dscr("s_v1", [10, NQ0, 128], BF16)
    S["att1"] = dscr("s_att1", [16, 128, NOWN], BF16) if start < 7 else din("s_att1", [16, 128, NOWN], BF16)
    S["x2T"] = dscr("s_x2T", [D, NOWN], F32)
    S["x3T"] = dscr("s_x3T", [D, NOWN], F32)
    S["h2"] = dscr("s_h2", [NOWN + 1, D], BF16)
    S["L"] = dscr("s_L", [NSLOT, 1], I32)
    S["Y"] = [dscr(f"s_Y{i}", [NSLOT + 1, D], F32) for i in range(2)]
    K.outT = nc.dram_tensor("outT", [D, NOWN], F32, kind="ExternalOutput").ap()
    K.Lb, K.h2b, K.Yb, K.x2b = Buf(), Buf(), Buf(), Buf()
    K.debug = debug
    K.S = S

    with ExitStack() as st:
        K.st = st
        K.ps = [st.enter_context(nc.psum_tensor(f"ps{i}", [128, 512], F32)) for i in range(7)]
        K.psb = [Buf(f"ps{i}") for i in range(7)]
        K.pst = st.enter_context(nc.psum_tensor("pst", [128, 1024], BF16))
        K.pstb = [Buf("pst0"), Buf("pst1")]
        K.pstw = [K.pst[:, 0:512], K.ps[6][:].bitcast(BF16)[:, 0:512]]
        K.pstwb = [K.pstb[0], K.psb[6]]
        K.ones = st.enter_context(nc.sbuf_tensor("ones", [128, 128], BF16))
        K.onesf = st.enter_context(nc.sbuf_tensor("onesf", [1, 128], F32))
        K.ident = st.enter_context(nc.sbuf_tensor("ident", [128, 128], BF16))
        K.mod = st.enter_context(nc.sbuf_tensor("modt", [128, 2, 6, NK, 2], F32))
        K.cb = Buf("consts")
        K.modb = Buf("mod")
        P.op("vector", lambda e: e.memset(K.ones[:], 1.0), writes=[K.cb])
        P.op("vector", lambda e: e.memset(K.onesf[:], 1.0), writes=[K.cb])
        P.op("gpsimd", lambda e: e.memset(K.ident[:], 1.0), writes=[K.cb])
        P.op("gpsimd", lambda e: e.affine_select(out=K.ident[:], in_=K.ident[:], pattern=[[-1, 128]],
                                                 compare_op=ALU.is_equal, fill=0.0, base=0, channel_multiplier=1),
             reads=[K.cb], writes=[K.cb])
        finals = []
        stage_ada(K)
        if debug:
            finals.append(P.dma("sync", S["mod"].rearrange("l p j k v -> p l j k v"), K.mod[:], reads=[K.modb]))
        P.barrier()
        if upto >= 2 and start < 5:
            stage_pre(K, 0)
        if upto >= 3 and start < 5:
            stage_attn0(K)
        if upto >= 4 and start < 5:
            stage_post0(K)
        if upto >= 5 and start < 7:
            stage_pre(K, 1)
        if upto >= 6 and start < 7:
            stage_attn1(K)
        if upto >= 7:
            stage_post1(K)
        if upto >= 8:
            stage_moe(K)
        if upto >= 9:
            finals += stage_final(K)
        P.emit(finals)
    return nc


def stage_ada(K):
    nc, P, I = K.nc, K.P, K.I
    with ExitStack() as st:
        T = lambda n, s, d: st.enter_context(nc.sbuf_tensor(n, s, d))
        cf = T("ada_cf", [128, NK, 2], F32)
        sg = T("ada_sg", [128, NK, 2], F32)
        cs = T("ada_cs", [128, NK, 2], BF16)
        wb = [T(f"ada_w{i}", [128, NK, 512], BF16) for i in range(2)]
        wbb = [Buf(), Buf()]
        raw = T("ada_raw", [128, 96, 2], F32)
        bt = T("ada_b", [128, 96], F32)
        gt = T("ada_g", [128, 2, NK], F32)
        b_c, b_raw, b_b, b_g = Buf(), Buf(), Buf(), Buf()
        P.dma("sync", cf[:], I["cT"], writes=[b_c])
        P.op("scalar", lambda e: e.activation(out=sg[:], in_=cf[:], func=AF.Sigmoid), reads=[b_c], writes=[b_raw])
        P.op("vector", lambda e: e.tensor_tensor(out=cs[:], in0=cf[:], in1=sg[:], op=ALU.mult), reads=[b_c, b_raw], writes=[b_c])
        mod = K.mod
        for l in (0, 1):
            P.dma("sync", bt[:], I[f"b_ada{l}"], writes=[b_b])
            P.dma("sync", gt[:, 0, :], I[f"g1_{l}"], writes=[b_g])
            P.dma("sync", gt[:, 1, :], I[f"g2_{l}"], writes=[b_g])
            wv = I[f"w_ada{l}"].rearrange("(k p) n -> p k n", p=128)
            for piece in range(24):
                w = wb[piece % 2]
                wbuf = wbb[piece % 2]
                P.dma("gpsimd", w[:], wv[:, :, piece * 512:(piece + 1) * 512], writes=[wbuf])
                pb = piece % 2
                ps, psb = K.ps[pb], K.psb[pb]
                for mm in range(4):
                    for k in range(NK):
                        P.op("tensor", lambda e, mm=mm, k=k, w=w, ps=ps: e.matmul(ps[:, mm * 2:mm * 2 + 2], w[:, k, mm * 128:(mm + 1) * 128], cs[:, k, :],
                                                                              start=(k == 0), stop=(k == NK - 1)),
                             reads=[wbuf, b_c], writes=[psb])
                P.op("vector", lambda e, piece=piece, ps=ps: e.tensor_copy(out=raw[:, piece * 4:(piece + 1) * 4, :], in_=ps[:, 0:8].rearrange("p (m v) -> p m v", v=2)),
                     reads=[psb], writes=[b_raw])
            for v in range(2):
                P.op("vector", lambda e, v=v: e.tensor_tensor(out=raw[:, :, v], in0=raw[:, :, v], in1=bt[:], op=ALU.add), reads=[b_raw, b_b], writes=[b_raw])
            rv = raw[:].rearrange("p (j k) v -> p j k v", j=6)
            for v in range(2):
                for (slot, src, gi) in ((0, 1, 0), (3, 4, 1)):
                    P.op("vector", lambda e, v=v, slot=slot, src=src, gi=gi, l=l: e.scalar_tensor_tensor(
                        out=mod[:, l, slot, :, v], in0=rv[:, src, :, v], scalar=1.0, in1=gt[:, gi, :], op0=ALU.add, op1=ALU.mult),
                        reads=[b_raw, b_g], writes=[K.modb])
                for (slot, src) in ((1, 0), (2, 2), (4, 3), (5, 5)):
                    P.op("vector", lambda e, v=v, slot=slot, src=src, l=l: e.tensor_copy(out=mod[:, l, slot, :, v], in_=rv[:, src, :, v]),
                         reads=[b_raw], writes=[K.modb])
        P.barrier()


def rms_modulate(K, xt, xb, ht, hb, tt, segs, l, slotA, slotB, tmp, tmpb, sq, sqb, rstd, rstdb, A=None, B=None, extra_reads=()):
    nc, P = K.nc, K.P
    mod = K.mod
    P.op("scalar", lambda e: e.activation(out=sq[:, :, :tt], in_=xt[:, :, :tt], func=AF.Square), reads=[xb], writes=[sqb])
    ps, psb = K.ps[6], K.psb[6]
    for k in range(NK):
        P.op("tensor", lambda e, k=k: e.matmul(ps[:, :tt], K.ones[:], sq[:, k, :tt], start=(k == 0), stop=(k == NK - 1)),
             reads=[sqb, K.cb], writes=[psb])
    P.op("scalar", lambda e: e.activation(out=rstd[:, :tt], in_=ps[:, :tt], func=AF.Sqrt, scale=1.0 / D, bias=EPS), reads=[psb], writes=[rstdb])
    P.op("vector", lambda e: e.reciprocal(out=rstd[:, :tt], in_=rstd[:, :tt]), reads=[rstdb], writes=[rstdb])
    for k in range(NK):
        tm, tmb = tmp[k % 2], tmpb[k % 2]
        P.op("vector", lambda e, k=k, tm=tm: e.tensor_tensor(out=tm[:, :tt], in0=xt[:, k, :tt], in1=rstd[:, :tt], op=ALU.mult),
             reads=[xb, rstdb], writes=[tmb])
        for (c0, c1, v) in segs:
            sA = mod[:, l, slotA, k, v:v + 1] if A is None else A(k, v)
            sB = mod[:, l, slotB, k, v:v + 1] if B is None else B(k, v)
            P.op("scalar", lambda e, k=k, c0=c0, c1=c1, tm=tm, sA=sA, sB=sB: e.activation(out=ht[:, k, c0:c1], in_=tm[:, c0:c1], func=AF.Identity, scale=sA, bias=sB),
                 reads=[tmb, K.modb] + list(extra_reads), writes=[hb])


def rope_pairs(K, eng_a, eng_b, src, dst, cos, sin, ng, npair, t1, t2, t3, t4, rb, wb_, tb):
    P = K.P
    sv = src.rearrange("p (g i t) -> p g i t", g=ng, i=npair, t=2)
    dv = dst.rearrange("p (g i t) -> p g i t", g=ng, i=npair, t=2)
    cb = cos.unsqueeze(1).to_broadcast([128, ng, npair])
    sb = sin.unsqueeze(1).to_broadcast([128, ng, npair])
    v = lambda t: t[:, :ng * npair].rearrange("p (g i) -> p g i", g=ng)
    P.op(eng_a, lambda e: e.tensor_tensor(out=v(t1), in0=sv[:, :, :, 0], in1=cb, op=ALU.mult), reads=rb, writes=[tb[0]])
    P.op(eng_a, lambda e: e.tensor_tensor(out=v(t2), in0=sv[:, :, :, 1], in1=sb, op=ALU.mult), reads=rb, writes=[tb[1]])
    P.op(eng_a, lambda e: e.tensor_tensor(out=dv[:, :, :, 0], in0=v(t1), in1=v(t2), op=ALU.subtract), reads=[tb[0], tb[1]], writes=wb_)
    P.op(eng_b, lambda e: e.tensor_tensor(out=v(t3), in0=sv[:, :, :, 0], in1=sb, op=ALU.mult), reads=rb, writes=[tb[2]])
    P.op(eng_b, lambda e: e.tensor_tensor(out=v(t4), in0=sv[:, :, :, 1], in1=cb, op=ALU.mult), reads=rb, writes=[tb[3]])
    P.op(eng_b, lambda e: e.tensor_tensor(out=dv[:, :, :, 1], in0=v(t3), in1=v(t4), op=ALU.add), reads=[tb[2], tb[3]], writes=wb_)


PRE_CFG = {
    0: {0: ("ropeA", "q", 0, None, None), 1: ("ropeA", "q", 4, None, None), 2: ("ropeB", "q", 8, 0, None), 3: ("ropeB", "q", 12, 0, None),
        4: ("ropeA", "k", 0, None, None), 5: ("ropeA", "k", 4, None, None), 6: ("v", None, None, None, 0), 7: ("v", None, None, None, 4),
        8: ("split", "k", 8, 1, 8)},
    1: {0: ("ropeB", "q", 0, None, None), 1: ("ropeB", "q", 4, None, None), 2: ("plain", "q", 8, None, None), 3: ("plain", "q", 12, None, None),
        4: ("split", "k", 0, None, 0), 5: ("plain", "k", 2, None, None), 6: ("plain", "k", 6, None, None), 7: ("v", None, None, None, 2),
        8: ("v", None, None, None, 6)},
}


def stage_pre(K, l):
    nc, P, I, S = K.nc, K.P, K.I, K.S
    cfg = PRE_CFG[l]
    tiles = []
    if l == 0:
        for t in range(4):
            tiles.append((t * 512, 512, True, [(0, 512, 0)], t * 512, t * 512, t * 512))
        tiles.append((2048, 256, True, [(0, 256, 0)], 2048, 2048, 2048))
        for t in range(3):
            c0 = 2304 + t * 512
            tiles.append((c0, 512, False, [(0, 512, 0)], c0, None, c0))
        tiles.append((2304 + 1536, 256, False, [(0, 256, 0)], 2304 + 1536, None, 2304 + 1536))
        tiles.append((SEQ, 256, True, [(0, 256, 1)], SEQ, NOWN + NHALO, SEQ))
        xv = I["xT"].rearrange("(k p) t -> p k t", p=128)
        qd, kd = S["qT0"], S["kT0"]
    else:
        for t in range(4):
            tiles.append((t * 512, 512, True, [(0, 512, 0)], t * 512, t * 512, t * 512))
        tiles.append((2048, 256, False, [(0, 256, 0)], 2048, None, 2048))
        tiles.append((2304, 256, False, [(0, 256, 1)], SEQ, None, 2304))
        xv = S["x1T"].rearrange("(k p) t -> p k t", p=128)
        qd, kd = S["qT1"], S["kT1"]
    wv = I[f"w_in{l}"].rearrange("(k p) n -> p k n", p=128)
    with ExitStack() as st:
        T = lambda n, s, d: st.enter_context(nc.sbuf_tensor(f"{n}_L{l}", s, d))
        xt = T("pre_x", [128, NK, 512], F32); xb = Buf()
        tmp = [T(f"pre_tmp{i}", [128, 512], F32) for i in range(2)]; tmpb = [Buf(), Buf()]
        sq = T("pre_sq", [128, NK, 512], BF16); sqb = Buf()
        ht = T("pre_h", [128, NK, 512], BF16); hb = Buf()
        rstd = T("pre_rstd", [128, 512], F32); rstdb = Buf()
        wt = [T(f"pre_w{i}", [128, NK, 512], BF16) for i in range(2)]; wtb = [Buf(), Buf()]
        ropet = T("pre_rope", [128, 4, 192], F32); ropeb = Buf()
        xs = [T(f"pre_xs{i}", [128, 512], F32) for i in range(2)]; xsb = [Buf(), Buf()]
        xn = [T(f"pre_xn{i}", [128, 512], F32) for i in range(2)]; xnb = [Buf(), Buf()]
        ob = [T(f"pre_ob{i}", [128, 512], BF16) for i in range(2)]; obb = [Buf(), Buf()]
        tt_ = [T(f"pre_t{i}", [128, 256], F32) for i in range(4)]; ttb = [Buf() for _ in range(4)]
        ss = T("pre_ss", [128, 4], F32); ssb = Buf()
        junk = T("pre_junk", [128, 128], F32); junkb = Buf()
        gq = T("pre_gq", [128, 2, 128], F32); gqb = Buf()
        qT = T("pre_qT", [128, 16, 512], BF16); qTb = Buf()
        kT = T("pre_kT", [128, 10, 512], BF16); kTb = Buf()
        vt = T("pre_v", [128, 10, 4, 128], BF16); vtb = Buf()
        if l == 0:
            P.dma("sync", gq[:, 0, :], I["hvec"][1:2, :].to_broadcast([128, 128]), writes=[gqb])
            P.dma("sync", gq[:, 1, :], I["hvec"][2:3, :].to_broadcast([128, 128]), writes=[gqb])
        cnt = {"w": 0, "e": 0}

        def epilogue(n, s, ps, psb):
            kind, dstk, h0, gi, vh0 = cfg[n]
            ei = cnt["e"]
            cnt["e"] += 1
            x_s, x_sb = xs[ei % 2], xsb[ei % 2]
            x_n, x_nb = xn[ei % 2], xnb[ei % 2]
            o_b, o_bb = ob[ei % 2], obb[ei % 2]
            pstv = K.pstw[ei % 2]
            pstb = K.pstwb[ei % 2]
            cosA, sinA = ropet[:, s, 0:32], ropet[:, s, 32:64]
            cosB, sinB = ropet[:, s, 64:128], ropet[:, s, 128:192]
            if kind == "v":
                P.op("scalar", lambda e: e.copy(out=vt[:, vh0:vh0 + 4, s, :], in_=ps[:].rearrange("p (h d) -> p h d", h=4)), reads=[psb], writes=[vtb])
                return
            P.op("scalar", lambda e: e.copy(out=x_s[:], in_=ps[:]), reads=[psb], writes=[x_sb])
            nh = 4
            if kind == "split":
                nh = 2
                P.op("vector", lambda e: e.tensor_copy(out=vt[:, vh0:vh0 + 2, s, :], in_=x_s[:, 256:512].rearrange("p (h d) -> p h d", h=2)), reads=[x_sb], writes=[vtb])
                kind = "ropeB"
            src, srcb = x_s, x_sb
            if gi is not None:
                P.op("vector", lambda e: e.memset(ss[:], 0.0), writes=[ssb])
                for h in range(nh):
                    P.op("scalar", lambda e, h=h: e.activation(out=junk[:], in_=x_s[:, h * 128:(h + 1) * 128], func=AF.Square, accum_out=ss[:, h:h + 1]),
                         reads=[x_sb], writes=[junkb, ssb])
                P.op("scalar", lambda e: e.activation(out=ss[:, :nh], in_=ss[:, :nh], func=AF.Sqrt, scale=1.0 / 128, bias=EPS), reads=[ssb], writes=[ssb])
                P.op("vector", lambda e: e.reciprocal(out=ss[:, :nh], in_=ss[:, :nh]), reads=[ssb], writes=[ssb])
                xv3 = x_s[:, :nh * 128].rearrange("p (h d) -> p h d", h=nh)
                xn3 = x_n[:, :nh * 128].rearrange("p (h d) -> p h d", h=nh)
                P.op("vector", lambda e: e.tensor_tensor(out=xn3, in0=xv3, in1=ss[:, :nh].unsqueeze(2).to_broadcast([128, nh, 128]), op=ALU.mult),
                     reads=[x_sb, ssb], writes=[x_nb])
                P.op("gpsimd", lambda e: e.tensor_tensor(out=xn3, in0=xn3, in1=gq[:, gi, :].unsqueeze(1).to_broadcast([128, nh, 128]), op=ALU.mult),
                     reads=[x_nb, gqb], writes=[x_nb])
                src, srcb = x_n, x_nb
            if kind == "ropeA":
                rope_pairs(K, "vector", "gpsimd", src[:], o_b[:], cosA, sinA, 8, 32, tt_[0], tt_[1], tt_[2], tt_[3], [srcb, ropeb], [o_bb], ttb)
                ntr = 4
            elif kind == "ropeB":
                rope_pairs(K, "vector", "gpsimd", src[:, :nh * 128], o_b[:, :nh * 128], cosB, sinB, nh, 64, tt_[0], tt_[1], tt_[2], tt_[3], [srcb, ropeb], [o_bb], ttb)
                ntr = nh
            else:
                P.op("vector", lambda e: e.tensor_copy(out=o_b[:], in_=src[:]), reads=[srcb], writes=[o_bb])
                ntr = 4
            for h in range(ntr):
                P.op("tensor", lambda e, h=h: e.transpose(pstv[:, h * 128:(h + 1) * 128], o_b[:, h * 128:(h + 1) * 128], K.ident[:]),
                     reads=[o_bb, K.cb], writes=[pstb])
            dst, dstb = (qT, qTb) if dstk == "q" else (kT, kTb)
            P.op("vector", lambda e: e.tensor_copy(out=dst[:, h0:h0 + ntr, s * 128:(s + 1) * 128], in_=pstv[:, :ntr * 128].rearrange("p (h t) -> p h t", h=ntr)),
                 reads=[pstb], writes=[dstb])

        def do_tile(t0, tt, need_q, segs, r0, qcol, kcol):
            nsub = tt // 128
            P.dma("sync", xt[:, :, :tt], xv[:, :, t0:t0 + tt], writes=[xb])
            P.dma("sync", ropet[:, :nsub, :], I["rope"][r0:r0 + tt, :].rearrange("(s p) c -> p s c", p=128), writes=[ropeb])
            rms_modulate(K, xt, xb, ht, hb, tt, segs, l, 0, 1, tmp, tmpb, sq, sqb, rstd, rstdb)
            for n in range(9):
                if not need_q and cfg[n][1] == "q":
                    continue
                w, wbuf = wt[cnt["w"] % 2], wtb[cnt["w"] % 2]
                cnt["w"] += 1
                P.dma("gpsimd", w[:], wv[:, :, n * 512:(n + 1) * 512], writes=[wbuf])
                for s in range(nsub):
                    bi = cnt["e"] % 4
                    ps, psb = K.ps[bi], K.psb[bi]
                    for k in range(NK):
                        P.op("tensor", lambda e, k=k, s=s, w=w, ps=ps: e.matmul(ps[:], ht[:, k, s * 128:(s + 1) * 128], w[:, k, :], start=(k == 0), stop=(k == NK - 1)),
                             reads=[hb, wbuf], writes=[psb])
                    epilogue(n, s, ps, psb)
            if need_q:
                P.dma("sync", qd[:, :, qcol:qcol + tt].rearrange("h p t -> p h t"), qT[:, :, :tt], reads=[qTb])
            P.dma("sync", kd[:, :, kcol:kcol + tt].rearrange("h p t -> p h t"), kT[:, :, :tt], reads=[kTb])
            if l == 0:
                P.dma("sync", S["v0"][:, :, kcol // 128:kcol // 128 + nsub, :].rearrange("h p s d -> p h s d"), vt[:, :, :nsub, :], reads=[vtb])
            else:
                for s in range(nsub):
                    P.dma("sync", S["v1"][:, kcol + s * 128:kcol + (s + 1) * 128, :].rearrange("h p d -> p h d"), vt[:, :, s, :], reads=[vtb])

        for tl in tiles:
            do_tile(*tl)
        P.barrier()


def stage_attn0(K):
    nc, P, I, S = K.nc, K.P, K.I, K.S
    ATT_SCALE = 128 ** -0.5
    A_SCALE = 64 ** -0.5
    with ExitStack() as st:
        T = lambda n, s, d: st.enter_context(nc.sbuf_tensor(n, s, d))
        kt = [T(f"at_k{i}", [128, NALL], BF16) for i in range(2)]; ktb = [Buf(), Buf()]
        vt = [T(f"at_v{i}", [128, NALL // 128, 128], BF16) for i in range(2)]; vtb = [Buf(), Buf()]
        qt = [T(f"at_q{i}", [128, 512], BF16) for i in range(2)]; qtb = [Buf(), Buf()]
        pt = [T(f"at_p{i}", [128, 512], BF16) for i in range(4)]; ptb = [Buf() for _ in range(4)]
        rec = [T(f"at_rec{i}", [128, 512], F32) for i in range(2)]; recb = [Buf(), Buf()]
        o0 = T("at_o0", [128, 512], F32); o0b = Buf()
        o1 = T("at_o1", [128, 512], F32); o1b = Buf()
        osq = T("at_osq", [128, 512], BF16); osqb = Buf()
        ot = [T(f"at_ot{i}", [128, 512], BF16) for i in range(2)]; otb = [Buf(), Buf()]
        lt = T("at_lt", [1, 4, 64], F32); ltb = Buf()
        lp = T("at_lp", [1, 2, 64], F32)
        l2 = T("at_l2", [1, 2], F32)
        l1 = T("at_l1", [1, 1], F32)
        nlam = T("at_nlam", [128, 1], F32); nlamb = Buf()
        sg = T("at_sg", [128, 1], F32); sgb = Buf()
        P.dma("sync", lt[:], I["lam"], writes=[ltb])
        P.op("vector", lambda e: e.tensor_tensor(out=lp[:], in0=lt[:, 0::2, :], in1=lt[:, 1::2, :], op=ALU.mult), reads=[ltb], writes=[ltb])
        P.op("vector", lambda e: e.tensor_reduce(out=l2[:], in_=lp[:], axis=AX.X, op=ALU.add), reads=[ltb], writes=[ltb])
        P.op("scalar", lambda e: e.activation(out=l2[:], in_=l2[:], func=AF.Exp), reads=[ltb], writes=[ltb])
        P.op("vector", lambda e: e.scalar_tensor_tensor(out=l1[:], in0=l2[:, 1:2], scalar=-LAMBDA_INIT_L0, in1=l2[:, 0:1], op0=ALU.add, op1=ALU.subtract), reads=[ltb], writes=[ltb])
        P.op("tensor", lambda e: e.matmul(K.ps[0][:, 0:1], K.onesf[:], l1[:], start=True, stop=True), reads=[ltb, K.cb], writes=[K.psb[0]])
        P.op("vector", lambda e: e.tensor_copy(out=nlam[:], in_=K.ps[0][:, 0:1]), reads=[K.psb[0]], writes=[nlamb])
        P.dma("sync", sg[:], I["hvec"][0:1, :].rearrange("o d -> d o"), writes=[sgb])
        P.op("vector", lambda e: e.tensor_scalar(out=sg[:], in0=sg[:], scalar1=1.0 - LAMBDA_INIT_L0, scalar2=None, op0=ALU.mult), reads=[sgb], writes=[sgb])

        qtiles = [(t * 512, 512, list(range(34))) for t in range(4)] + [(2048, 256, list(range(34))), (2304, 256, [32, 33])]
        cnt = {"q": 0, "s": 0, "p": 0, "o": 0}
        def do_tile(kk, kkb, vv, vvb, qh, q0, tt, chunks, is_a, ncomp, scale):
            q_, q_b = qt[cnt["q"] % 2], qtb[cnt["q"] % 2]
            cnt["q"] += 1
            P.dma("sync", q_[:, :tt], S["qT0"][qh, :, q0:q0 + tt], writes=[q_b])
            jobs = [(kc, c) for kc in chunks for c in range(ncomp)]
            sbank = {}

            def issue_s(j):
                kc, c = jobs[j]
                bi = cnt["s"] % 3
                cnt["s"] += 1
                sbank[j] = bi
                if is_a:
                    lo, hi = c * 64, (c + 1) * 64
                else:
                    lo, hi = 0, 128
                P.op("tensor", lambda e, kc=kc, lo=lo, hi=hi, bi=bi: e.matmul(K.ps[bi][:, :tt], kk[lo:hi, kc * 128:(kc + 1) * 128], q_[lo:hi, :tt], start=True, stop=True),
                     reads=[kkb, q_b], writes=[K.psb[bi]])
            LA = 2
            for j in range(min(LA, len(jobs))):
                issue_s(j)
            for j, (kc, c) in enumerate(jobs):
                bi = sbank[j]
                p_, p_b = pt[cnt["p"] % 4], ptb[cnt["p"] % 4]
                cnt["p"] += 1
                P.op("scalar", lambda e, bi=bi, p_=p_: e.activation(out=p_[:, :tt], in_=K.ps[bi][:, :tt], func=AF.Exp, scale=scale), reads=[K.psb[bi]], writes=[p_b])
                if j + LA < len(jobs):
                    issue_s(j + LA)
                first, last = (kc == chunks[0]), (kc == chunks[-1])
                P.op("tensor", lambda e, kc=kc, c=c, p_=p_, first=first, last=last: e.matmul(K.ps[3 + c][:, :tt], vv[:, kc, :], p_[:, :tt], start=first, stop=last),
                     reads=[vvb, p_b], writes=[K.psb[3 + c]])
                P.op("tensor", lambda e, c=c, p_=p_, first=first, last=last: e.matmul(K.ps[5 + c][:, :tt], K.ones[:], p_[:, :tt], start=first, stop=last),
                     reads=[K.cb, p_b], writes=[K.psb[5 + c]])
            o_, o_b = ot[cnt["o"] % 2], otb[cnt["o"] % 2]
            cnt["o"] += 1
            for c in range(ncomp):
                P.op("vector", lambda e, c=c: e.reciprocal(out=rec[c][:, :tt], in_=K.ps[5 + c][:, :tt]), reads=[K.psb[5 + c]], writes=[recb[c]])
            if not is_a:
                P.op("vector", lambda e, o_=o_: e.tensor_tensor(out=o_[:, :tt], in0=K.ps[3][:, :tt], in1=rec[0][:, :tt], op=ALU.mult), reads=[K.psb[3], recb[0]], writes=[o_b])
            else:
                P.op("vector", lambda e: e.tensor_tensor(out=o0[:, :tt], in0=K.ps[3][:, :tt], in1=rec[0][:, :tt], op=ALU.mult), reads=[K.psb[3], recb[0]], writes=[o0b])
                P.op("vector", lambda e: e.tensor_tensor(out=o1[:, :tt], in0=K.ps[4][:, :tt], in1=rec[1][:, :tt], op=ALU.mult), reads=[K.psb[4], recb[1]], writes=[o1b])
                P.op("vector", lambda e: e.scalar_tensor_tensor(out=o0[:, :tt], in0=o1[:, :tt], scalar=nlam[:, 0:1], in1=o0[:, :tt], op0=ALU.mult, op1=ALU.add),
                     reads=[o0b, o1b, nlamb], writes=[o0b])
                P.op("scalar", lambda e: e.activation(out=osq[:, :tt], in_=o0[:, :tt], func=AF.Square), reads=[o0b], writes=[osqb])
                bi = cnt["s"] % 3
                cnt["s"] += 1
                P.op("tensor", lambda e, bi=bi: e.matmul(K.ps[bi][:, :tt], K.ones[:], osq[:, :tt], start=True, stop=True), reads=[osqb, K.cb], writes=[K.psb[bi]])
                P.op("scalar", lambda e, bi=bi: e.activation(out=rec[0][:, :tt], in_=K.ps[bi][:, :tt], func=AF.Sqrt, scale=1.0 / 128, bias=EPS), reads=[K.psb[bi]], writes=[recb[0]])
                P.op("vector", lambda e: e.reciprocal(out=rec[0][:, :tt], in_=rec[0][:, :tt]), reads=[recb[0]], writes=[recb[0]])
                P.op("vector", lambda e, o_=o_: e.scalar_tensor_tensor(out=o_[:, :tt], in0=o0[:, :tt], scalar=sg[:, 0:1], in1=rec[0][:, :tt], op0=ALU.mult, op1=ALU.mult),
                     reads=[o0b, sgb, recb[0]], writes=[o_b])
            P.dma("sync", S["att0"][qh, :, q0:q0 + tt], o_[:, :tt], reads=[o_b])

        for kvh in range(10):
            kk, kkb = kt[kvh % 2], ktb[kvh % 2]
            vv, vvb = vt[kvh % 2], vtb[kvh % 2]
            P.dma("sync", kk[:], S["kT0"][kvh], writes=[kkb])
            P.dma("sync", vv[:], S["v0"][kvh], writes=[vvb])
            is_a = kvh < 8
            qheads = [kvh] if is_a else list(range(8 + (kvh - 8) * 4, 12 + (kvh - 8) * 4))
            ncomp = 2 if is_a else 1
            scale = A_SCALE if is_a else ATT_SCALE
            for qh in qheads:
                for (q0, tt, chunks) in qtiles:
                    do_tile(kk, kkb, vv, vvb, qh, q0, tt, chunks, is_a, ncomp, scale)
        P.barrier()


def stage_post0(K):
    nc, P, I, S = K.nc, K.P, K.I, K.S
    l = 0
    NJ = D_FF // 128
    xv = I["xT"].rearrange("(k p) t -> p k t", p=128)
    wov = I["w_out0"].rearrange("(k p) n -> p k n", p=128)
    wgv = I["ffn_wg"].rearrange("(k p) n -> p k n", p=128)
    wuv = I["ffn_wu"].rearrange("(k p) n -> p k n", p=128)
    wdv = I["ffn_wd"].rearrange("(j p) n -> p j n", p=128)
    x1v = S["x1T"].rearrange("(k p) t -> p k t", p=128)
    with ExitStack() as st:
        T = lambda n, s, d: st.enter_context(nc.sbuf_tensor(n, s, d))
        big = T("po_big", [128, NJ, 512], BF16); bigb = Buf()
        xt = T("po_x", [128, NK, 512], F32); xb = Buf()
        tmp = [T(f"po_tmp{i}", [128, 512], F32) for i in range(2)]; tmpb = [Buf(), Buf()]
        ht = T("po_h", [128, NK, 512], BF16); hb = Buf()
        rstd = T("po_rstd", [128, 512], F32); rstdb = Buf()
        wt = [T(f"po_w{i}", [128, NK, 512], BF16) for i in range(2)]; wtb = [Buf(), Buf()]
        wd = [T(f"po_wd{i}", [128, NJ, 256], BF16) for i in range(2)]; wdb = [Buf(), Buf()]
        sl = [T(f"po_sl{i}", [128, 512], F32) for i in range(2)]; slb = [Buf(), Buf()]
        att = big[:, 0:NK, :]
        sq = big[:, NK:2 * NK, :]
        cnt = {"w": 0, "wd": 0, "ps": 0, "sl": 0}

        def do_tile(q0, xsrc, segs):
            P.dma("sync", att, S["att0"][:, :, q0:q0 + 512].rearrange("h p t -> p h t"), writes=[bigb])
            for (c0, c1, s0) in xsrc:
                P.dma("sync", xt[:, :, c0:c1], xv[:, :, s0:s0 + (c1 - c0)], writes=[xb])
            for pc in range(4):
                w, wbuf = wt[cnt["w"] % 2], wtb[cnt["w"] % 2]
                cnt["w"] += 1
                P.dma("gpsimd", w[:], wov[:, :, pc * 512:(pc + 1) * 512], writes=[wbuf])
                for mm in range(4):
                    m = pc * 4 + mm
                    bi = cnt["ps"] % 4
                    cnt["ps"] += 1
                    for k in range(NK):
                        P.op("tensor", lambda e, k=k, mm=mm, w=w, bi=bi: e.matmul(K.ps[bi][:], w[:, k, mm * 128:(mm + 1) * 128], att[:, k, :], start=(k == 0), stop=(k == NK - 1)),
                             reads=[wbuf, bigb], writes=[K.psb[bi]])
                    for (c0, c1, v) in segs:
                        P.op("vector", lambda e, m=m, bi=bi, c0=c0, c1=c1, v=v: e.scalar_tensor_tensor(out=xt[:, m, c0:c1], in0=K.ps[bi][:, c0:c1], scalar=K.mod[:, l, 2, m, v:v + 1],
                                                                                                  in1=xt[:, m, c0:c1], op0=ALU.mult, op1=ALU.add),
                             reads=[K.psb[bi], xb, K.modb], writes=[xb])
            if CUT == 1:
                P.dma("sync", x1v[:, :, q0:q0 + 512], xt[:], reads=[xb]); return
            rms_modulate(K, xt, xb, ht, hb, 512, segs, l, 3, 4, tmp, tmpb, sq, bigb, rstd, rstdb)
            if CUT == 2:
                P.dma("sync", x1v[:, :, q0:q0 + 512], xt[:], reads=[xb]); return
            for jp in range(NJ // 2):
                w, wbuf = wt[cnt["w"] % 2], wtb[cnt["w"] % 2]
                cnt["w"] += 1
                P.dma("gpsimd", w[:, :, 0:256], wgv[:, :, jp * 256:(jp + 1) * 256], writes=[wbuf])
                P.dma("gpsimd", w[:, :, 256:512], wuv[:, :, jp * 256:(jp + 1) * 256], writes=[wbuf])
                for jj in range(2):
                    j = jp * 2 + jj
                    bg = (cnt["ps"] % 2) * 2
                    cnt["ps"] += 1
                    for k in range(NK):
                        P.op("tensor", lambda e, k=k, jj=jj, w=w, bg=bg: e.matmul(K.ps[bg][:], w[:, k, jj * 128:(jj + 1) * 128], ht[:, k, :], start=(k == 0), stop=(k == NK - 1)),
                             reads=[wbuf, hb], writes=[K.psb[bg]])
                    for k in range(NK):
                        P.op("tensor", lambda e, k=k, jj=jj, w=w, bg=bg: e.matmul(K.ps[bg + 1][:], w[:, k, 256 + jj * 128:256 + (jj + 1) * 128], ht[:, k, :], start=(k == 0), stop=(k == NK - 1)),
                             reads=[wbuf, hb], writes=[K.psb[bg + 1]])
                    s_, s_b = sl[cnt["sl"] % 2], slb[cnt["sl"] % 2]
                    cnt["sl"] += 1
                    P.op("scalar", lambda e, bg=bg, s_=s_: e.activation(out=s_[:], in_=K.ps[bg][:], func=AF.Silu), reads=[K.psb[bg]], writes=[s_b])
                    P.op("vector", lambda e, bg=bg, s_=s_, j=j: e.tensor_tensor(out=big[:, j, :], in0=s_[:], in1=K.ps[bg + 1][:], op=ALU.mult),
                         reads=[s_b, K.psb[bg + 1]], writes=[bigb])
            if CUT == 3:
                P.dma("sync", x1v[:, :, q0:q0 + 512], xt[:], reads=[xb]); return
            for pc in range(8):
                w, wbuf = wd[cnt["wd"] % 2], wdb[cnt["wd"] % 2]
                cnt["wd"] += 1
                for jq in range(4):
                    P.dma("gpsimd", w[:, jq * 11:(jq + 1) * 11, :], wdv[:, jq * 11:(jq + 1) * 11, pc * 256:(pc + 1) * 256], writes=[wbuf])
                for mm in range(2):
                    m = pc * 2 + mm
                    bi = 4 + cnt["ps"] % 2
                    cnt["ps"] += 1
                    for j in range(NJ):
                        P.op("tensor", lambda e, j=j, mm=mm, w=w, bi=bi: e.matmul(K.ps[bi][:], w[:, j, mm * 128:(mm + 1) * 128], big[:, j, :], start=(j == 0), stop=(j == NJ - 1)),
                             reads=[wbuf, bigb], writes=[K.psb[bi]])
                    for (c0, c1, v) in segs:
                        P.op("vector", lambda e, m=m, bi=bi, c0=c0, c1=c1, v=v: e.scalar_tensor_tensor(out=xt[:, m, c0:c1], in0=K.ps[bi][:, c0:c1], scalar=K.mod[:, l, 5, m, v:v + 1],
                                                                                                  in1=xt[:, m, c0:c1], op0=ALU.mult, op1=ALU.add),
                             reads=[K.psb[bi], xb, K.modb], writes=[xb])
            P.dma("sync", x1v[:, :, q0:q0 + 512], xt[:], reads=[xb])

        for t in range(4):
            do_tile(t * 512, [(0, 512, t * 512)], [(0, 512, 0)])
        do_tile(2048, [(0, 256, 2048), (256, 512, SEQ)], [(0, 256, 0), (256, 512, 1)])
        P.barrier()


def stage_attn1(K):
    nc, P, I, S = K.nc, K.P, K.I, K.S
    ATT_SCALE = 128 ** -0.5
    with ExitStack() as st:
        T = lambda n, s, d: st.enter_context(nc.sbuf_tensor(n, s, d))
        kt = [T(f"a1_k{i}", [128, NQ0], BF16) for i in range(2)]; ktb = [Buf(), Buf()]
        vc = [T(f"a1_vc{i}", [128, 20, 128], BF16) for i in range(2)]; vcb = [Buf(), Buf()]
        vd = [T(f"a1_vd{i}", [64, 40, 128], BF16) for i in range(2)]; vdb = [Buf(), Buf()]
        qt = [T(f"a1_q{i}", [128, NOWN], BF16) for i in range(2)]; qtb = [Buf(), Buf()]
        pt = [T(f"a1_p{i}", [128, 512], BF16) for i in range(4)]; ptb = [Buf() for _ in range(4)]
        mk = T("a1_mask", [128, 8, 512], BF16); mkb = Buf()
        rec = T("a1_rec", [128, 512], F32); recb = Buf()
        ot = [T(f"a1_ot{i}", [128, 512], BF16) for i in range(2)]; otb = [Buf(), Buf()]
        sk = T("a1_sk", [1, 8], F32); skb = Buf()
        es = T("a1_es", [128, 8], F32); esb = Buf()
        bg = [T(f"a1_bg{i}", [64, 8, 64], F32) for i in range(2)]; bgb = [Buf(), Buf()]
        be = [T(f"a1_be{i}", [64, 12, 64], F32) for i in range(2)]; beb = [Buf(), Buf()]
        tA = [T(f"a1_tA{i}", [64, 512], F32) for i in range(2)]; tAb = [Buf(), Buf()]
        tB = [T(f"a1_tB{i}", [64, 256], F32) for i in range(2)]; tBb = [Buf(), Buf()]
        pA = [T(f"a1_pA{i}", [64, 512], BF16) for i in range(2)]; pAb = [Buf(), Buf()]
        pB = [T(f"a1_pB{i}", [64, 512], BF16) for i in range(2)]; pBb = [Buf(), Buf()]
        P.dma("gpsimd", mk[:], I["maskC"].rearrange("m p q -> p m q"), writes=[mkb])
        P.dma("sync", sk[:], I["sinks"], writes=[skb])
        P.op("scalar", lambda e: e.activation(out=sk[:], in_=sk[:], func=AF.Exp), reads=[skb], writes=[skb])
        P.op("tensor", lambda e: e.matmul(K.ps[0][:, 0:8], K.onesf[:], sk[:], start=True, stop=True), reads=[skb, K.cb], writes=[K.psb[0]])
        P.op("vector", lambda e: e.tensor_copy(out=es[:], in_=K.ps[0][:, 0:8]), reads=[K.psb[0]], writes=[esb])
        cnt = {"q": 0, "s": 0, "p": 0, "o": 0, "kv": 0}

        def c_tile(kk, kkb, vv, vvb, q_, q_b, qh, t):
            n0 = 4 * t
            chunks = []
            for r in range(-1, 5):
                kb = n0 + r
                if kb < 0:
                    chunks.append((2176, 17, 6))
                elif kb > 15:
                    chunks.append((2048, 16, 7))
                else:
                    chunks.append((kb * 128, kb, r + 1))
            chunks += [(2304, 18, None), (2432, 19, None)]
            sbank = {}

            def issue_s(j):
                col = chunks[j][0]
                bi = cnt["s"] % 3
                cnt["s"] += 1
                sbank[j] = bi
                P.op("tensor", lambda e: e.matmul(K.ps[bi][:], kk[:, col:col + 128], q_[:, t * 512:(t + 1) * 512], start=True, stop=True),
                     reads=[kkb, q_b], writes=[K.psb[bi]])
            LA = 2
            for j in range(LA):
                issue_s(j)
            for j, (col, vch, mi) in enumerate(chunks):
                bi = sbank[j]
                p_, p_b = pt[cnt["p"] % 4], ptb[cnt["p"] % 4]
                cnt["p"] += 1
                P.op("scalar", lambda e, bi=bi, p_=p_: e.activation(out=p_[:], in_=K.ps[bi][:], func=AF.Exp, scale=ATT_SCALE), reads=[K.psb[bi]], writes=[p_b])
                if mi is not None:
                    P.op("vector", lambda e, p_=p_, mi=mi: e.tensor_tensor(out=p_[:], in0=p_[:], in1=mk[:, mi, :], op=ALU.mult), reads=[p_b, mkb], writes=[p_b])
                if j + LA < len(chunks):
                    issue_s(j + LA)
                first, last = (j == 0), (j == len(chunks) - 1)
                P.op("tensor", lambda e, vch=vch, p_=p_, first=first, last=last: e.matmul(K.ps[3][:], vv[:, vch, :], p_[:], start=first, stop=last),
                     reads=[vvb, p_b], writes=[K.psb[3]])
                P.op("tensor", lambda e, p_=p_, first=first, last=last: e.matmul(K.ps[5][:], K.ones[:], p_[:], start=first, stop=last),
                     reads=[K.cb, p_b], writes=[K.psb[5]])
            o_, o_b = ot[cnt["o"] % 2], otb[cnt["o"] % 2]
            cnt["o"] += 1
            P.op("vector", lambda e: e.tensor_scalar(out=rec[:], in0=K.ps[5][:], scalar1=es[:, qh:qh + 1], scalar2=None, op0=ALU.add), reads=[K.psb[5], esb], writes=[recb])
            P.op("vector", lambda e: e.reciprocal(out=rec[:], in_=rec[:]), reads=[recb], writes=[recb])
            P.op("vector", lambda e: e.tensor_tensor(out=o_[:], in0=K.ps[3][:], in1=rec[:], op=ALU.mult), reads=[K.psb[3], recb], writes=[o_b])
            P.dma("sync", S["att1"][qh, :, t * 512:(t + 1) * 512], o_[:], reads=[o_b])

        for kvh in range(2):
            i = cnt["kv"] % 2
            cnt["kv"] += 1
            P.dma("sync", kt[i][:], S["kT1"][kvh], writes=[ktb[i]])
            P.dma("sync", vc[i][:], S["v1"][kvh].rearrange("(c p) d -> p c d", p=128), writes=[vcb[i]])
            for g in range(4):
                qh = kvh * 4 + g
                qi = cnt["q"] % 2
                cnt["q"] += 1
                P.dma("sync", qt[qi][:], S["qT1"][qh], writes=[qtb[qi]])
                for t in range(4):
                    c_tile(kt[i], ktb[i], vc[i], vcb[i], qt[qi], qtb[qi], qh, t)

        def d_head(h):
            i = cnt["kv"] % 2
            cnt["kv"] += 1
            kk, kkb, vv, vvb = kt[i], ktb[i], vd[i], vdb[i]
            P.dma("sync", kk[:], S["kT1"][2 + h], writes=[kkb])
            vsrc = S["v1"][2 + h].rearrange("(r p) d -> p r d", p=64)
            P.dma("sync", vv[:, 0:20, :], vsrc[:, 0:20, :], writes=[vvb])
            P.dma("sync", vv[:, 20:40, :], vsrc[:, 20:40, :], writes=[vvb])
            qi = cnt["q"] % 2
            cnt["q"] += 1
            q_, q_b = qt[qi], qtb[qi]
            P.dma("sync", q_[:], S["qT1"][8 + h], writes=[q_b])
            bgt, bgtb = bg[h % 2], bgb[h % 2]
            P.dma("sync", bgt[:], I["biasD"][h], writes=[bgtb])
            state = {}

            def row_scores(r):
                edge = (r < 4) or (r > 27)
                if not edge:
                    own = list(range(r - 4, r + 4))
                    bias, biasb = bgt, bgtb
                else:
                    own = list(range(0, 8)) if r < 4 else list(range(24, 32))
                    er = r if r < 4 else r - 24
                    bias, biasb = be[er % 2], beb[er % 2]
                    P.dma("sync", bias[:], I["biasE"][er, h], writes=[biasb])
                sa = cnt["s"] % 2
                cnt["s"] += 1
                qs = q_[:, r * 64:(r + 1) * 64]
                for c, sr in enumerate(own):
                    P.op("tensor", lambda e, c=c, sr=sr: e.matmul(K.ps[sa][0:64, c * 64:(c + 1) * 64], kk[:, sr * 64:(sr + 1) * 64], qs, start=True, stop=True),
                         reads=[kkb, q_b], writes=[K.psb[sa]])
                extra = ([32, 33, 34, 35] if edge else []) + [36, 37, 38, 39]
                off = 0 if edge else 4
                for c, sr in enumerate(extra):
                    cc = c + off
                    P.op("tensor", lambda e, cc=cc, sr=sr: e.matmul(K.ps[2][0:64, cc * 64:(cc + 1) * 64], kk[:, sr * 64:(sr + 1) * 64], qs, start=True, stop=True),
                         reads=[kkb, q_b], writes=[K.psb[2]])
                ta, tab = tA[r % 2], tAb[r % 2]
                tb, tbb = tB[r % 2], tBb[r % 2]
                pa, pab = pA[r % 2], pAb[r % 2]
                pb, pbb = pB[r % 2], pBb[r % 2]
                P.op("vector", lambda e: e.scalar_tensor_tensor(out=ta[:], in0=K.ps[sa][0:64, :], scalar=ATT_SCALE, in1=bias[:, 0:8, :].rearrange("p c q -> p (c q)"),
                                                                op0=ALU.mult, op1=ALU.add), reads=[K.psb[sa], biasb], writes=[tab])
                P.op("scalar", lambda e: e.activation(out=pa[:], in_=ta[:], func=AF.Exp), reads=[tab], writes=[pab])
                if edge:
                    P.op("vector", lambda e: e.scalar_tensor_tensor(out=tb[:], in0=K.ps[2][0:64, 0:256], scalar=ATT_SCALE, in1=bias[:, 8:12, :].rearrange("p c q -> p (c q)"),
                                                                    op0=ALU.mult, op1=ALU.add), reads=[K.psb[2], biasb], writes=[tbb])
                    P.op("scalar", lambda e: e.activation(out=pb[:, 0:256], in_=tb[:], func=AF.Exp), reads=[tbb], writes=[pbb])
                P.op("scalar", lambda e: e.activation(out=pb[:, 256:512], in_=K.ps[2][0:64, 256:512], func=AF.Exp, scale=ATT_SCALE), reads=[K.psb[2]], writes=[pbb])
                state[r] = (own, extra, off, pa, pab, pb, pbb)

            def row_pv(r):
                own, extra, off, pa, pab, pb, pbb = state.pop(r)
                grp = r // 8
                po, pob = K.ps[3 + grp % 2], K.psb[3 + grp % 2]
                pss, pssb = K.ps[5 + grp % 2], K.psb[5 + grp % 2]
                cs = slice((r % 8) * 64, (r % 8) * 64 + 64)
                items = [(sr, pa, pab, c) for c, sr in enumerate(own)] + [(sr, pb, pbb, c + off) for c, sr in enumerate(extra)]
                for n_, (sr, pp, ppb, c) in enumerate(items):
                    first, last = (n_ == 0), (n_ == len(items) - 1)
                    P.op("tensor", lambda e, sr=sr, pp=pp, c=c, first=first, last=last: e.matmul(po[:, cs], vv[:, sr, :], pp[:, c * 64:(c + 1) * 64], start=first, stop=last),
                         reads=[vvb, ppb], writes=[pob])
                    P.op("tensor", lambda e, pp=pp, c=c, first=first, last=last: e.matmul(pss[:, cs], K.ones[0:64, :], pp[:, c * 64:(c + 1) * 64], start=first, stop=last),
                         reads=[K.cb, ppb], writes=[pssb])
                if r % 8 == 7:
                    o_, o_b = ot[cnt["o"] % 2], otb[cnt["o"] % 2]
                    cnt["o"] += 1
                    P.op("vector", lambda e: e.reciprocal(out=rec[:], in_=pss[:]), reads=[pssb], writes=[recb])
                    P.op("vector", lambda e: e.tensor_tensor(out=o_[:], in0=po[:], in1=rec[:], op=ALU.mult), reads=[pob, recb], writes=[o_b])
                    P.dma("sync", S["att1"][8 + h, :, grp * 512:(grp + 1) * 512], o_[:], reads=[o_b])

            row_scores(0)
            for r in range(32):
                if r + 1 < 32:
                    row_scores(r + 1)
                row_pv(r)

        for h in range(8):
            d_head(h)
        P.barrier()


CAP = 1024
NSLOT = N_EXP * CAP
NJH = D_EXP // 128 // 2


def stage_post1(K):
    nc, P, I, S = K.nc, K.P, K.I, K.S
    l = 1
    x1v = S["x1T"].rearrange("(k p) t -> p k t", p=128)
    x2v = S["x2T"].rearrange("(k p) t -> p k t", p=128)
    wov = I["w_out1"].rearrange("(k p) n -> p k n", p=128)
    st = K.st
    TP = lambda n, s_, d: st.enter_context(nc.sbuf_tensor(n, s_, d))
    K.gate = TP("gate12", [128, 16, 2], F32); K.gateb = Buf()
    K.sloti = TP("slot12i", [128, 16, 2], I32); K.slotib = Buf()
    with ExitStack() as st2:
        T = lambda n, s_, d: st2.enter_context(nc.sbuf_tensor(n, s_, d))
        att = T("p1_att", [128, NK, 512], BF16); attb = Buf()
        sq = T("p1_sq", [128, NK, 512], BF16); sqb = Buf()
        xt = T("p1_x", [128, NK, 512], F32); xb = Buf()
        tmp = [T(f"p1_tmp{i}", [128, 512], F32) for i in range(2)]; tmpb = [Buf(), Buf()]
        ht = T("p1_h", [128, NK, 512], BF16); hb = Buf()
        rstd = T("p1_rstd", [128, 512], F32); rstdb = Buf()
        wt = [T(f"p1_w{i}", [128, NK, 512], BF16) for i in range(2)]; wtb = [Buf(), Buf()]
        wr = T("p1_wr", [128, NK, 8], BF16); wrb = Buf()
        htok = [T(f"p1_htok{i}", [128, D], BF16) for i in range(2)]; htokb = [Buf(), Buf()]
        U = T("p1_U", [128, 128], BF16); Ub = Buf()
        lg = T("p1_lg", [128, 8], F32); lgb = Buf()
        mx = T("p1_mx", [128, 8], F32); mxb = Buf()
        ind = T("p1_ind", [128, 3, 8], F32); indb = Buf()
        indh = T("p1_indh", [128, 8], BF16); indhb = Buf()
        base = T("p1_base", [128, 8], F32); baseb = Buf()
        pos = T("p1_pos", [128, 8], F32); posb = Buf()
        val = T("p1_val", [128, 8], F32); valb = Buf()
        eoff = T("p1_eoff", [128, 8], F32); eoffb = Buf()
        eoffi = T("p1_eoffi", [128, 8], I32)
        pidi = T("p1_pidi", [128, 1], I32)
        pidf = T("p1_pidf", [128, 1], F32); pidb = Buf()
        tokf = T("p1_tokf", [128, 1], F32)
        toki = [T(f"p1_toki{i}", [128, 1], I32) for i in range(2)]; tokb = [Buf(), Buf()]
        slf = T("p1_slf", [128, 2], F32); slfb = Buf()
        junk8 = T("p1_junk8", [128, 8], F32); junk8b = Buf()
        dl = T("p1_dl", [128, 1], F32); dlb = Buf()
        cfill = T("p1_cfill", [128, NSLOT // 128], I32); cfb = Buf()
        zrow = T("p1_zrow", [1, D], BF16); zrb = Buf()
        zrowf = T("p1_zrowf", [1, D], F32)
        Lb, h2b, Yb = K.Lb, K.h2b, K.Yb
        P.op("gpsimd", lambda e: e.memset(U[:], 1.0), writes=[Ub])
        P.op("gpsimd", lambda e: e.affine_select(out=U[:], in_=U[:], pattern=[[1, 128]], compare_op=ALU.is_gt, fill=0.0, base=0, channel_multiplier=-1), reads=[Ub], writes=[Ub])
        P.op("gpsimd", lambda e: e.iota(eoffi[:], pattern=[[CAP, 8]], base=0, channel_multiplier=0), writes=[eoffb])
        P.op("vector", lambda e: e.tensor_copy(out=eoff[:], in_=eoffi[:]), reads=[eoffb], writes=[eoffb])
        P.op("gpsimd", lambda e: e.iota(pidi[:], pattern=[[0, 1]], base=0, channel_multiplier=1), writes=[pidb])
        P.op("vector", lambda e: e.tensor_copy(out=pidf[:], in_=pidi[:]), reads=[pidb], writes=[pidb])
        P.op("vector", lambda e: e.memset(base[:], 0.0), writes=[baseb])
        P.op("gpsimd", lambda e: e.iota(cfill[:], pattern=[[0, NSLOT // 128]], base=NOWN, channel_multiplier=0), writes=[cfb])
        P.dma("sync", S["L"][0:NSLOT, :].rearrange("(p c) o -> p (c o)", p=128), cfill[:], reads=[cfb], writes=[Lb])
        P.op("vector", lambda e: e.memset(zrow[:], 0.0), writes=[zrb])
        P.op("vector", lambda e: e.memset(zrowf[:], 0.0), writes=[zrb])
        P.dma("sync", S["h2"][NOWN:NOWN + 1, :], zrow[:], reads=[zrb], writes=[h2b])
        for hf in range(2):
            P.dma("sync", S["Y"][hf][NSLOT:NSLOT + 1, :], zrowf[:], reads=[zrb], writes=[Yb])
        P.dma("gpsimd", wr[:], I["router"].rearrange("(k p) e -> p k e", p=128), writes=[wrb])
        cnt = {"w": 0, "ps": 0, "h": 0, "t": 0}

        def do_tile(t):
            q0 = t * 512
            P.dma("sync", att[:], S["att1"][:, :, q0:q0 + 512].rearrange("h p t -> p h t"), writes=[attb])
            P.dma("sync", xt[:], x1v[:, :, q0:q0 + 512], writes=[xb])
            for pc in range(4):
                w, wbuf = wt[cnt["w"] % 2], wtb[cnt["w"] % 2]
                cnt["w"] += 1
                P.dma("gpsimd", w[:], wov[:, :, pc * 512:(pc + 1) * 512], writes=[wbuf])
                for mm in range(4):
                    m = pc * 4 + mm
                    bi = cnt["ps"] % 4
                    cnt["ps"] += 1
                    for k in range(NK):
                        P.op("tensor", lambda e, k=k, mm=mm, w=w, bi=bi: e.matmul(K.ps[bi][:], w[:, k, mm * 128:(mm + 1) * 128], att[:, k, :], start=(k == 0), stop=(k == NK - 1)),
                             reads=[wbuf, attb], writes=[K.psb[bi]])
                    P.op("vector", lambda e, m=m, bi=bi: e.scalar_tensor_tensor(out=xt[:, m, :], in0=K.ps[bi][:], scalar=K.mod[:, l, 2, m, 0:1], in1=xt[:, m, :], op0=ALU.mult, op1=ALU.add),
                         reads=[K.psb[bi], xb, K.modb], writes=[xb])
            P.dma("sync", x2v[:, :, q0:q0 + 512], xt[:], reads=[xb], writes=[K.x2b])
            rms_modulate(K, xt, xb, ht, hb, 512, [(0, 512, 0)], l, 3, 4, tmp, tmpb, sq, sqb, rstd, rstdb)
            for s_ in range(4):
                if CUT == 11:
                    break
                sub = t * 4 + s_
                hk, hkb = htok[cnt["h"] % 2], htokb[cnt["h"] % 2]
                cnt["h"] += 1
                for q4 in range(4):
                    pw, pwb = K.pstw[q4 % 2], K.pstwb[q4 % 2]
                    for kk in range(4):
                        k = q4 * 4 + kk
                        P.op("tensor", lambda e, k=k, kk=kk, pw=pw, s_=s_: e.transpose(pw[:, kk * 128:(kk + 1) * 128], ht[:, k, s_ * 128:(s_ + 1) * 128], K.ident[:]), reads=[hb, K.cb], writes=[pwb])
                    if q4 % 2 == 0:
                        P.op("vector", lambda e, q4=q4, hk=hk, pw=pw: e.tensor_copy(out=hk[:, q4 * 512:(q4 + 1) * 512], in_=pw), reads=[pwb], writes=[hkb])
                    else:
                        P.op("vector", lambda e, q4=q4, hk=hk, pw=pw: e.tensor_copy(out=hk[:, q4 * 512:(q4 + 1) * 512], in_=pw), reads=[pwb], writes=[hkb])
                if not os.environ.get("KNOH2"):
                    P.dma("sync", S["h2"][q0 + s_ * 128:q0 + (s_ + 1) * 128, :], hk[:], reads=[hkb], writes=[h2b])
                if CUT == 12:
                    continue
                for k in range(NK):
                    P.op("tensor", lambda e, k=k, s_=s_: e.matmul(K.ps[4][:, 0:8], ht[:, k, s_ * 128:(s_ + 1) * 128], wr[:, k, :], start=(k == 0), stop=(k == NK - 1)), reads=[hb, wrb], writes=[K.psb[4]])
                P.op("vector", lambda e: e.tensor_copy(out=lg[:], in_=K.ps[4][:, 0:8]), reads=[K.psb[4]], writes=[lgb])
                P.op("vector", lambda e: e.max(out=mx[:], in_=lg[:]), reads=[lgb], writes=[mxb])
                for c in range(2):
                    P.op("vector", lambda e, c=c: e.tensor_scalar(out=ind[:, c, :], in0=lg[:], scalar1=mx[:, c:c + 1], scalar2=None, op0=ALU.is_equal), reads=[lgb, mxb], writes=[indb])
                P.op("vector", lambda e: e.tensor_tensor(out=indh[:], in0=ind[:, 0, :], in1=ind[:, 1, :], op=ALU.add), reads=[indb], writes=[indhb])
                P.op("vector", lambda e: e.tensor_tensor(out=dl[:], in0=mx[:, 0:1], in1=mx[:, 1:2], op=ALU.subtract), reads=[mxb], writes=[dlb])
                P.op("scalar", lambda e, sub=sub: e.activation(out=K.gate[:, sub, 0:1], in_=dl[:], func=AF.Sigmoid), reads=[dlb], writes=[K.gateb])
                P.op("vector", lambda e, sub=sub: e.tensor_scalar(out=K.gate[:, sub, 1:2], in0=K.gate[:, sub, 0:1], scalar1=-1.0, scalar2=1.0, op0=ALU.mult, op1=ALU.add), reads=[K.gateb], writes=[K.gateb])
                P.op("tensor", lambda e: e.matmul(K.ps[5][:, 0:8], U[:], indh[:], start=True, stop=True), reads=[Ub, indhb], writes=[K.psb[5]])
                P.op("tensor", lambda e: e.matmul(K.ps[5][:, 8:16], K.ones[:], indh[:], start=True, stop=True), reads=[K.cb, indhb], writes=[K.psb[5]])
                P.op("vector", lambda e: e.tensor_tensor(out=pos[:], in0=K.ps[5][:, 0:8], in1=base[:], op=ALU.add), reads=[K.psb[5], baseb], writes=[posb])
                P.op("vector", lambda e: e.tensor_tensor(out=base[:], in0=K.ps[5][:, 8:16], in1=base[:], op=ALU.add), reads=[K.psb[5], baseb], writes=[baseb])
                P.op("vector", lambda e: e.tensor_scalar(out=ind[:, 2, :], in0=pos[:], scalar1=float(CAP), scalar2=None, op0=ALU.is_lt), reads=[posb], writes=[indb])
                P.op("vector", lambda e: e.scalar_tensor_tensor(out=val[:], in0=pos[:], scalar=-float(NSLOT), in1=eoff[:], op0=ALU.add, op1=ALU.add), reads=[posb, eoffb], writes=[valb])
                P.op("vector", lambda e: e.tensor_tensor(out=val[:], in0=val[:], in1=ind[:, 2, :], op=ALU.mult), reads=[valb, indb], writes=[valb])
                P.op("vector", lambda e: e.tensor_scalar(out=val[:], in0=val[:], scalar1=float(NSLOT), scalar2=None, op0=ALU.add), reads=[valb], writes=[valb])
                for c in range(2):
                    P.op("vector", lambda e, c=c: e.tensor_tensor(out=junk8[:], in0=val[:], in1=ind[:, c, :], op=ALU.mult), reads=[valb, indb], writes=[junk8b])
                    P.op("vector", lambda e, c=c: e.tensor_reduce(out=slf[:, c:c + 1], in_=junk8[:], axis=AX.X, op=ALU.add), reads=[junk8b], writes=[slfb])
                P.op("vector", lambda e, sub=sub: e.tensor_copy(out=K.sloti[:, sub, :], in_=slf[:]), reads=[slfb], writes=[K.slotib])
                tk, tkb = toki[cnt["t"] % 2], tokb[cnt["t"] % 2]
                cnt["t"] += 1
                P.op("vector", lambda e, sub=sub: e.tensor_scalar(out=tokf[:], in0=pidf[:], scalar1=float(sub * 128), scalar2=None, op0=ALU.add), reads=[pidb], writes=[tkb])
                P.op("vector", lambda e, tk=tk: e.tensor_copy(out=tk[:], in_=tokf[:]), reads=[tkb], writes=[tkb])
                for c in range(2):
                    if CUT == 13:
                        continue
                    P._add("gpsimd", lambda e, c=c, sub=sub, tk=tk: e.indirect_dma_start(out=S["L"][:, :], out_offset=bass.IndirectOffsetOnAxis(ap=K.sloti[:, sub, c:c + 1], axis=0),
                                                                                        in_=tk[:], in_offset=None, bounds_check=NSLOT - 1, oob_is_err=False),
                           [K.slotib, tkb], [Lb], True)

        for t in range(4):
            if CUT == 10:
                break
            do_tile(t)
        P.barrier()


def stage_moe(K):
    nc, P, I, S = K.nc, K.P, K.I, K.S
    Lb, h2b, Yb = K.Lb, K.h2b, K.Yb
    with ExitStack() as st2:
        T = lambda n, s_, d: st2.enter_context(nc.sbuf_tensor(n, s_, d))
        hT = T("mo_hT", [128, NK, CAP], BF16); hTb = Buf()
        aT = T("mo_aT", [128, NJH, CAP], BF16); aTb = Buf()
        wt = [T(f"mo_w{i}", [128, NK, 512], BF16) for i in range(2)]; wtb = [Buf(), Buf()]
        wd = [T(f"mo_wd{i}", [128, NJH, 256], BF16) for i in range(2)]; wdb = [Buf(), Buf()]
        hg = [T(f"mo_hg{i}", [128, D], BF16) for i in range(2)]; hgb = [Buf(), Buf()]
        idx = [T(f"mo_idx{i}", [128, 1], I32) for i in range(2)]; idxb = [Buf(), Buf()]
        sl = [T(f"mo_sl{i}", [128, 512], F32) for i in range(2)]; slb = [Buf(), Buf()]
        ys = [T(f"mo_ys{i}", [128, 256], F32) for i in range(2)]; ysb = [Buf(), Buf()]
        cnt = {"w": 0, "wd": 0, "ps": 0, "g": 0, "sl": 0, "y": 0, "pst": 0}

        def expert(ex):
            wgv = I["exp_wg"][ex].rearrange("(k p) n -> p k n", p=128)
            wuv = I["exp_wu"][ex].rearrange("(k p) n -> p k n", p=128)
            wdv = I["exp_wd"][ex].rearrange("(j p) n -> p j n", p=128)
            for sb in range(CAP // 128):
                gi = cnt["g"] % 2
                cnt["g"] += 1
                r0 = ex * CAP + sb * 128
                P.dma("sync", idx[gi][:], S["L"][r0:r0 + 128, :], reads=[Lb], writes=[idxb[gi]])
                P._add("gpsimd", lambda e, gi=gi: e.indirect_dma_start(out=hg[gi][:], out_offset=None, in_=S["h2"][:, :],
                                                                       in_offset=bass.IndirectOffsetOnAxis(ap=idx[gi][:, 0:1], axis=0)),
                       [idxb[gi], h2b], [hgb[gi]], True)
                for q4 in range(4):
                    pw, pwb = K.pstw[q4 % 2], K.pstwb[q4 % 2]
                    for kk in range(4):
                        k = q4 * 4 + kk
                        P.op("tensor", lambda e, k=k, kk=kk, gi=gi, pw=pw: e.transpose(pw[:, kk * 128:(kk + 1) * 128], hg[gi][:, k * 128:(k + 1) * 128], K.ident[:]),
                             reads=[hgb[gi], K.cb], writes=[pwb])
                    if q4 % 2 == 0:
                        P.op("vector", lambda e, sb=sb, q4=q4, pw=pw: e.tensor_copy(out=hT[:, q4 * 4:(q4 + 1) * 4, sb * 128:(sb + 1) * 128], in_=pw.rearrange("p (k t) -> p k t", k=4)),
                             reads=[pwb], writes=[hTb])
                    else:
                        P.op("vector", lambda e, sb=sb, q4=q4, pw=pw: e.tensor_copy(out=hT[:, q4 * 4:(q4 + 1) * 4, sb * 128:(sb + 1) * 128], in_=pw.rearrange("p (k t) -> p k t", k=4)),
                             reads=[pwb], writes=[hTb])
            for hf in range(2):
                for jp in range(NJH // 2):
                    w, wbuf = wt[cnt["w"] % 2], wtb[cnt["w"] % 2]
                    cnt["w"] += 1
                    c0 = hf * (D_EXP // 2) + jp * 256
                    P.dma("gpsimd", w[:, :, 0:256], wgv[:, :, c0:c0 + 256], writes=[wbuf])
                    P.dma("gpsimd", w[:, :, 256:512], wuv[:, :, c0:c0 + 256], writes=[wbuf])
                    for jj in range(2):
                        j = jp * 2 + jj
                        for tt in range(CAP // 512):
                            bg = (cnt["ps"] % 2) * 2
                            cnt["ps"] += 1
                            for k in range(NK):
                                P.op("tensor", lambda e, k=k, jj=jj, w=w, bg=bg, tt=tt: e.matmul(K.ps[bg][:], w[:, k, jj * 128:(jj + 1) * 128], hT[:, k, tt * 512:(tt + 1) * 512], start=(k == 0), stop=(k == NK - 1)),
                                     reads=[wbuf, hTb], writes=[K.psb[bg]])
                            for k in range(NK):
                                P.op("tensor", lambda e, k=k, jj=jj, w=w, bg=bg, tt=tt: e.matmul(K.ps[bg + 1][:], w[:, k, 256 + jj * 128:256 + (jj + 1) * 128], hT[:, k, tt * 512:(tt + 1) * 512], start=(k == 0), stop=(k == NK - 1)),
                                     reads=[wbuf, hTb], writes=[K.psb[bg + 1]])
                            s_, s_b = sl[cnt["sl"] % 2], slb[cnt["sl"] % 2]
                            cnt["sl"] += 1
                            P.op("scalar", lambda e, bg=bg, s_=s_: e.activation(out=s_[:], in_=K.ps[bg][:], func=AF.Silu), reads=[K.psb[bg]], writes=[s_b])
                            P.op("vector", lambda e, bg=bg, s_=s_, j=j, tt=tt: e.tensor_tensor(out=aT[:, j, tt * 512:(tt + 1) * 512], in0=s_[:], in1=K.ps[bg + 1][:], op=ALU.mult),
                                 reads=[s_b, K.psb[bg + 1]], writes=[aTb])
                for ng in range(8):
                    w, wbuf = wd[cnt["wd"] % 2], wdb[cnt["wd"] % 2]
                    cnt["wd"] += 1
                    for jq in range(2):
                        P.dma("gpsimd", w[:, jq * 14:(jq + 1) * 14, :], wdv[:, hf * NJH + jq * 14:hf * NJH + (jq + 1) * 14, ng * 256:(ng + 1) * 256], writes=[wbuf])
                    for sb in range(CAP // 128):
                        bi = 4 + cnt["ps"] % 2
                        cnt["ps"] += 1
                        for j in range(NJH):
                            P.op("tensor", lambda e, j=j, sb=sb, w=w, bi=bi: e.matmul(K.ps[bi][:, 0:256], aT[:, j, sb * 128:(sb + 1) * 128], w[:, j, :], start=(j == 0), stop=(j == NJH - 1)),
                                 reads=[wbuf, aTb], writes=[K.psb[bi]])
                        y_, y_b = ys[cnt["y"] % 2], ysb[cnt["y"] % 2]
                        if cnt["y"] % 2 == 0:
                            P.op("vector", lambda e, y_=y_, bi=bi: e.tensor_copy(out=y_[:], in_=K.ps[bi][:, 0:256]), reads=[K.psb[bi]], writes=[y_b])
                        else:
                            P.op("scalar", lambda e, y_=y_, bi=bi: e.copy(out=y_[:], in_=K.ps[bi][:, 0:256]), reads=[K.psb[bi]], writes=[y_b])
                        cnt["y"] += 1
                        r0 = ex * CAP + sb * 128
                        P.dma("sync", S["Y"][hf][r0:r0 + 128, ng * 256:(ng + 1) * 256], y_[:], reads=[y_b], writes=[Yb])

        for ex in range(N_EXP):
            expert(ex)
        P.barrier()


def stage_final(K):
    nc, P, I, S = K.nc, K.P, K.I, K.S
    l = 1
    x2v = S["x2T"].rearrange("(k p) t -> p k t", p=128)
    outv = K.outT.rearrange("(k p) t -> p k t", p=128)
    finals = []
    with ExitStack() as st2:
        T = lambda n, s_, d: st2.enter_context(nc.sbuf_tensor(n, s_, d))
        yg = [T(f"fi_yg{i}", [128, D], F32) for i in range(4)]; ygb = [Buf() for _ in range(4)]
        acc = T("fi_acc", [128, D], F32); accb = Buf()
        xt = T("fi_x", [128, NK, 512], F32); xb = Buf()
        sq = T("fi_sq", [128, NK, 512], BF16); sqb = Buf()
        ot = T("fi_o", [128, NK, 512], F32); otb = Buf()
        tmp = [T(f"fi_tmp{i}", [128, 512], F32) for i in range(2)]; tmpb = [Buf(), Buf()]
        rstd = T("fi_rstd", [128, 512], F32); rstdb = Buf()
        identf = T("fi_identf", [128, 128], F32); idfb = Buf()
        gf = T("fi_gf", [128, 2, NK], F32); gfb = Buf()
        P.op("gpsimd", lambda e: e.memset(identf[:], 1.0), writes=[idfb])
        P.op("gpsimd", lambda e: e.affine_select(out=identf[:], in_=identf[:], pattern=[[-1, 128]], compare_op=ALU.is_equal, fill=0.0, base=0, channel_multiplier=1), reads=[idfb], writes=[idfb])
        P.op("vector", lambda e: e.memset(gf[:], 0.0), writes=[gfb])
        P.dma("sync", gf[:, 0, :], I["gfin"], writes=[gfb])
        for t in range(4):
            q0 = t * 512
            P.dma("sync", xt[:], x2v[:, :, q0:q0 + 512], reads=[K.x2b], writes=[xb])
            for s_ in range(4):
                sub = t * 4 + s_
                for c in range(2):
                    for hf in range(2):
                        i = c * 2 + hf
                        P._add("gpsimd", lambda e, i=i, c=c, hf=hf, sub=sub: e.indirect_dma_start(out=yg[i][:], out_offset=None, in_=S["Y"][hf][:, :],
                                                                                                in_offset=bass.IndirectOffsetOnAxis(ap=K.sloti[:, sub, c:c + 1], axis=0)),
                               [K.slotib, K.Yb], [ygb[i]], True)
                P.op("vector", lambda e: e.tensor_tensor(out=yg[0][:], in0=yg[0][:], in1=yg[1][:], op=ALU.add), reads=[ygb[0], ygb[1]], writes=[ygb[0]])
                P.op("gpsimd", lambda e: e.tensor_tensor(out=yg[2][:], in0=yg[2][:], in1=yg[3][:], op=ALU.add), reads=[ygb[2], ygb[3]], writes=[ygb[2]])
                P.op("vector", lambda e, sub=sub: e.tensor_scalar(out=acc[:], in0=yg[0][:], scalar1=K.gate[:, sub, 0:1], scalar2=None, op0=ALU.mult), reads=[ygb[0], K.gateb], writes=[accb])
                P.op("vector", lambda e, sub=sub: e.scalar_tensor_tensor(out=acc[:], in0=yg[2][:], scalar=K.gate[:, sub, 1:2], in1=acc[:], op0=ALU.mult, op1=ALU.add),
                     reads=[ygb[2], K.gateb, accb], writes=[accb])
                for grp in range(4):
                    bi = grp % 4
                    for mm in range(4):
                        m = grp * 4 + mm
                        P.op("tensor", lambda e, m=m, mm=mm, bi=bi: e.transpose(K.ps[bi][:, mm * 128:(mm + 1) * 128], acc[:, m * 128:(m + 1) * 128], identf[:]),
                             reads=[accb, idfb], writes=[K.psb[bi]])
                    for mm in range(4):
                        m = grp * 4 + mm
                        P.op("vector", lambda e, m=m, mm=mm, bi=bi, s_=s_: e.scalar_tensor_tensor(out=xt[:, m, s_ * 128:(s_ + 1) * 128], in0=K.ps[bi][:, mm * 128:(mm + 1) * 128],
                                                                                              scalar=K.mod[:, l, 5, m, 0:1], in1=xt[:, m, s_ * 128:(s_ + 1) * 128], op0=ALU.mult, op1=ALU.add),
                             reads=[K.psb[bi], xb, K.modb], writes=[xb])
            rms_modulate(K, xt, xb, ot, otb, 512, [(0, 512, 0)], l, 0, 0, tmp, tmpb, sq, sqb, rstd, rstdb,
                         A=lambda k, v: gf[:, 0, k:k + 1], B=lambda k, v: gf[:, 1, k:k + 1], extra_reads=[gfb])
            if K.debug:
                P.dma("sync", K.S["x3T"].rearrange("(k p) t -> p k t", p=128)[:, :, q0:q0 + 512], xt[:], reads=[xb])
            finals.append(P.dma("sync", outv[:, :, q0:q0 + 512], ot[:], reads=[otb]))
    return finals


def prep_core_inputs(inputs):
    x, c, ctx, c_ctx = inputs["x"], inputs["c"], inputs["ctx"], inputs["c_ctx"]
    shared = {}
    for l in (0, 1):
        shared[f"w_ada{l}"] = np.ascontiguousarray(inputs[f"l{l}_w_ada"], dtype=np.float32)
        shared[f"b_ada{l}"] = _fm(inputs[f"l{l}_b_ada"])
        shared[f"g1_{l}"] = _fm(inputs[f"l{l}_norm1_g"])
        shared[f"g2_{l}"] = _fm(inputs[f"l{l}_norm2_g"])
        shared[f"w_in{l}"] = np.ascontiguousarray(inputs[f"l{l}_w_in"], dtype=np.float32)
        shared[f"w_out{l}"] = np.ascontiguousarray(inputs[f"l{l}_w_out"], dtype=np.float32)
    shared["ffn_wg"] = np.ascontiguousarray(inputs["l0_ffn_w_gate"], dtype=np.float32)
    shared["ffn_wu"] = np.ascontiguousarray(inputs["l0_ffn_w_up"], dtype=np.float32)
    shared["ffn_wd"] = np.ascontiguousarray(inputs["l0_ffn_w_down"], dtype=np.float32)
    shared["router"] = np.ascontiguousarray(inputs["l1_router"], dtype=np.float32)
    shared["exp_wg"] = np.ascontiguousarray(inputs["l1_exp_w_gate"], dtype=np.float32)
    shared["exp_wu"] = np.ascontiguousarray(inputs["l1_exp_w_up"], dtype=np.float32)
    shared["exp_wd"] = np.ascontiguousarray(inputs["l1_exp_w_down"], dtype=np.float32)
    shared["gfin"] = _fm(inputs["final_norm_g"])
    shared["sinks"] = inputs["l1_sinks"].reshape(1, 8).astype(np.float32)
    shared["biasD"] = _bias_d(inputs["l1_rpb"].astype(np.float32))
    bias_e = [_bias_e(inputs["l1_rpb"].astype(np.float32), hf) for hf in range(2)]
    mask_c = [_mask_c(hf) for hf in range(2)]
    shared["lam"] = np.stack([inputs["l0_lam_q1"], inputs["l0_lam_k1"], inputs["l0_lam_q2"], inputs["l0_lam_k2"]])[None].astype(np.float32)
    shared["hvec"] = np.stack([inputs["l0_subln_g"], inputs["l0_q_norm_g"], inputs["l0_k_norm_g"]]).astype(np.float32)
    maps = []
    for ci in range(8):
        b, half = ci // 2, ci % 2
        order = core_token_order(half)
        m = dict(shared)
        xt = np.empty((D, NALL), np.float32)
        xt[:, :SEQ] = x[b][order].T
        xt[:, SEQ:] = ctx[b].T
        m["xT"] = xt
        cT = np.stack([_fm(c[b]), _fm(c_ctx)], axis=-1)
        m["cT"] = np.ascontiguousarray(cT, dtype=np.float32)
        m["biasE"] = bias_e[half]
        m["maskC"] = mask_c[half]
        m["rope"] = _rope_tables(np.concatenate([order, -np.ones(NCTX, np.int64)]))
        maps.append(m)
    return maps


_NC_CACHE = {}


def kernel(**inputs):
    inputs = {k: np.asarray(v) for k, v in inputs.items()}
    maps = prep_core_inputs(inputs)
    if "nc" not in _NC_CACHE:
        _NC_CACHE["nc"] = build_program()
    nc = _NC_CACHE["nc"]
    res = run_bass_kernel_spmd(nc, maps, core_ids=list(range(8)))
    out = np.empty((4, SEQ, D), np.float32)
    for ci in range(8):
        b, half = ci // 2, ci % 2
        out[b, half * NOWN:(half + 1) * NOWN] = res.results[ci]["outT"].T
    return out
```

```python
import math
import os
CUT = int(os.environ.get('KCUT', '99'))
from contextlib import ExitStack

import numpy as np
import concourse.bass as bass
import concourse.mybir as mybir
from concourse.bass_utils import run_bass_kernel_spmd

F32 = mybir.dt.float32
BF16 = mybir.dt.bfloat16
I32 = mybir.dt.int32
AF = mybir.ActivationFunctionType
ALU = mybir.AluOpType
AX = mybir.AxisListType

ENGS = ("tensor", "vector", "scalar", "gpsimd", "sync")
SEM_WRAP = 20000
N_DMA_SLOTS = 6

D = 2048
NK = 16
SEQ = 4096
NCTX = 256
NOWN = 2048
NHALO = 256
NQ0 = NOWN + NHALO + NCTX
NALL = SEQ + NCTX
EPS = 1e-6
LAMBDA_INIT_L0 = 0.8 - 0.6 * math.exp(-0.3 * 0)
D_FF = 5632
N_EXP = 8
D_EXP = 7168


class Buf:
    __slots__ = ("name", "last_w", "readers")

    def __init__(self, name=""):
        self.name = name
        self.last_w = None
        self.readers = []


class Op:
    __slots__ = ("eng", "fn", "waits", "is_dma", "slot", "use", "needs_inc", "clock", "semval")

    def __init__(self):
        self.needs_inc = False
        self.semval = None
        self.waits = []


class Prog:
    def __init__(self, nc, same_engine_sync=True):
        self.nc = nc
        self.ops = {e: [] for e in ENGS}
        self.known = {e: {} for e in ENGS}
        self.dma_slot_use = {}
        self.dma_slot_last = {}
        self.dma_rr = {e: 0 for e in ENGS}
        self.same_engine_sync = same_engine_sync
        self.last_ev = {e: None for e in ENGS}

    def _add(self, eng, fn, reads, writes, is_dma, extra_deps=()):
        op = Op()
        op.eng = eng
        op.fn = fn
        op.is_dma = is_dma
        deps = list(extra_deps)
        for b in reads:
            if b.last_w is not None:
                deps.append(b.last_w)
        for b in writes:
            if b.last_w is not None:
                deps.append(b.last_w)
            deps.extend(b.readers)
        if is_dma:
            slot = self.dma_rr[eng]
            self.dma_rr[eng] = (slot + 1) % N_DMA_SLOTS
            key = ("dma", eng, slot)
            use = self.dma_slot_use.get(key, 0) + 1
            self.dma_slot_use[key] = use
            prev = self.dma_slot_last.get(key)
            if prev is not None:
                deps.append(prev)
            op.slot = slot
            op.use = use
            ev = (key, use, op)
            self.dma_slot_last[key] = ev
        else:
            ev = (eng, len(self.ops[eng]), op)
        known = self.known[eng]
        changed = False
        best = {}
        for (k, i, dop) in deps:
            if known.get(k, -1) >= i:
                continue
            if k == eng and not is_dma:
                if eng == "tensor" or not self.same_engine_sync:
                    continue
            if not changed:
                known = dict(known)
                changed = True
            if k not in best or best[k][1] < i:
                best[k] = (k, i, dop)
            for kk, vv in dop.clock.items():
                if known.get(kk, -1) < vv:
                    known[kk] = vv
            if known.get(k, -1) < i:
                known[k] = i
        op.waits = list(best.values())
        for (k, i, dop) in op.waits:
            dop.needs_inc = True
        if changed:
            self.known[eng] = known
        if is_dma:
            c = dict(known)
            c[ev[0]] = ev[1]
            op.clock = c
        else:
            c = dict(known)
            c[eng] = ev[1]
            op.clock = c
            if fn is not None:
                self.last_ev[eng] = ev
        self.ops[eng].append(op)
        for b in reads:
            b.readers.append(ev)
        for b in writes:
            b.last_w = ev
            b.readers = []
        return ev

    def op(self, eng, fn, reads=(), writes=()):
        return self._add(eng, fn, reads, writes, False)

    def dma(self, eng, out, in_, reads=(), writes=(), **kw):
        return self._add(eng, lambda e: e.dma_start(out=out, in_=in_, **kw), reads, writes, True)

    def barrier(self):
        evs = [ev for ev in self.last_ev.values() if ev is not None] + list(self.dma_slot_last.values())
        for e in ENGS:
            if self.ops[e]:
                self._add(e, None, (), (), False, extra_deps=evs)

    def emit(self, final_events=()):
        nc = self.nc
        self._add("sync", None, (), (), False, extra_deps=list(final_events))
        n_sems = {}
        for e in ENGS:
            c = 0
            for o in self.ops[e]:
                if o.is_dma:
                    continue
                if o.needs_inc:
                    o.semval = (c // SEM_WRAP, c % SEM_WRAP + 1)
                    c += 1
            n_sems[e] = c // SEM_WRAP + 1
        with ExitStack() as st:
            sems = {}
            for e in ENGS:
                sems[e] = [st.enter_context(nc.semaphore(f"s_{e}_{i}")) for i in range(n_sems[e])]
            dsems = {}
            for key in self.dma_slot_use:
                dsems[key] = st.enter_context(nc.semaphore(f"d_{key[1]}_{key[2]}"))
            block = st.enter_context(nc.Block())

            def make(e):
                def body(eng):
                    pending_inc = None
                    for o in self.ops[e]:
                        for (k, i, dop) in o.waits:
                            if isinstance(k, tuple):
                                eng.wait_ge(dsems[k], 16 * i)
                            else:
                                si, v = dop.semval
                                eng.wait_ge(sems[k][si], v)
                        if o.fn is None:
                            assert not o.needs_inc
                            continue
                        inst = o.fn(eng)
                        if o.is_dma:
                            inst.then_inc(dsems[("dma", e, o.slot)], 16)
                        elif o.needs_inc:
                            inst.then_inc(sems[e][o.semval[0]], 1)
                return body

            for e in ENGS:
                if self.ops[e]:
                    getattr(block, e)(make(e))


def _rope_tables(pos_tok):
    n = len(pos_tok)
    out = np.zeros((n, 192), np.float32)
    valid = pos_tok >= 0
    t = np.where(valid, pos_tok, 0)
    pos = np.stack([t // 64, t % 64], axis=-1).astype(np.float32)
    col = 0
    for dim in (64, 128):
        n_pairs = dim // 4
        freq = (np.float32(10000.0) ** (-np.arange(n_pairs, dtype=np.float32) / np.float32(n_pairs))).astype(np.float32)
        ang = (pos[:, :, None] * freq).reshape(n, 2 * n_pairs).astype(np.float32)
        c = np.where(valid[:, None], np.cos(ang), 1.0).astype(np.float32)
        s = np.where(valid[:, None], np.sin(ang), 0.0).astype(np.float32)
        out[:, col:col + 2 * n_pairs] = c
        out[:, col + 2 * n_pairs:col + 4 * n_pairs] = s
        col += 4 * n_pairs
    return out


def _fm(v):
    return np.ascontiguousarray(v.reshape(-1, 128).T)


NEG = -30000.0


def _mask_c(half):
    m = np.zeros((8, 128, 512), np.float32)
    kj = np.arange(128)[:, None]
    qi = np.arange(128)[None, :]
    for r in range(-1, 5):
        for j in range(4):
            rel = r - j
            if rel == 0:
                blk = np.ones((128, 128), np.float32)
            elif rel == -1:
                blk = (kj >= qi).astype(np.float32)
            elif rel == 1:
                blk = (kj <= qi).astype(np.float32)
            else:
                continue
            m[r + 1, :, j * 128:(j + 1) * 128] = blk
    if half == 1:
        m[6] = m[0]
    else:
        m[7] = m[5]
    return m


def _na_bias_tile(rpb, drow):
    qc = np.arange(64)[None, :]
    kc = np.arange(64)[:, None]
    cstart = np.clip(qc - 8, 0, 48)
    ok = (kc >= cstart) & (kc < cstart + 16)
    dcol = np.clip(kc - qc + 15, 0, 30)
    if drow < 0 or drow > 14:
        return np.full((8, 64, 64), NEG, np.float32)
    b = rpb[:, drow][:, dcol]
    return np.where(ok[None], b, np.float32(NEG)).astype(np.float32)


def _bias_d(rpb):
    out = np.empty((8, 64, 8, 64), np.float32)
    for di in range(8):
        out[:, :, di, :] = _na_bias_tile(rpb, 3 + di)
    return out


def _bias_e(rpb, half):
    out = np.full((8, 8, 64, 12, 64), NEG, np.float32)
    for er in range(8):
        r = er if er < 4 else 24 + er
        R = r + 32 * half
        rs = min(max(R - 4, 0), 56)
        own = list(range(0, 8)) if er < 4 else list(range(24, 32))
        cand_global = [32 * half + o for o in own]
        halo_global = [32 + i for i in range(4)] if half == 0 else [28 + i for i in range(4)]
        for c, Rk in enumerate(cand_global + halo_global):
            if rs <= Rk < rs + 8:
                out[er, :, :, c, :] = _na_bias_tile(rpb, Rk - R + 7)
    return out


def core_token_order(half):
    own = np.arange(half * NOWN, (half + 1) * NOWN)
    halo = np.arange(NOWN, NOWN + NHALO) if half == 0 else np.arange(NOWN - NHALO, NOWN)
    mask = np.ones(SEQ, bool)
    mask[own] = False
    mask[halo] = False
    rest = np.nonzero(mask)[0]
    return np.concatenate([own, halo, rest])


class Ctx:
    pass


def build_program(upto=99, debug=False, start=0):
    nc = bass.Bass("TRN2", target_bir_lowering=False)
    P = Prog(nc)
    K = Ctx()
    K.nc, K.P = nc, P

    def din(name, shape, dt=F32):
        return nc.dram_tensor(name, list(shape), dt, kind="ExternalInput").ap()

    def dscr(name, shape, dt):
        kind = "ExternalOutput" if debug else "Internal"
        return nc.dram_tensor(name, list(shape), dt, kind=kind).ap()

    I = {}
    I["xT"] = din("xT", [D, NALL])
    I["cT"] = din("cT", [128, NK, 2])
    I["rope"] = din("rope", [NALL, 192])
    for l in (0, 1):
        I[f"w_ada{l}"] = din(f"w_ada{l}", [D, 6 * D])
        I[f"b_ada{l}"] = din(f"b_ada{l}", [128, 96])
        I[f"g1_{l}"] = din(f"g1_{l}", [128, NK])
        I[f"g2_{l}"] = din(f"g2_{l}", [128, NK])
        I[f"w_in{l}"] = din(f"w_in{l}", [D, 4608])
        I[f"w_out{l}"] = din(f"w_out{l}", [D, D])
    if start < 5:
        I["ffn_wg"] = din("ffn_wg", [D, D_FF])
        I["ffn_wu"] = din("ffn_wu", [D, D_FF])
        I["ffn_wd"] = din("ffn_wd", [D_FF, D])
    I["maskC"] = din("maskC", [8, 128, 512])
    I["sinks"] = din("sinks", [1, 8])
    I["biasD"] = din("biasD", [8, 64, 8, 64])
    I["biasE"] = din("biasE", [8, 8, 64, 12, 64])
    I["router"] = din("router", [D, N_EXP])
    if upto >= 8:
        I["exp_wg"] = din("exp_wg", [N_EXP, D, D_EXP])
        I["exp_wu"] = din("exp_wu", [N_EXP, D, D_EXP])
        I["exp_wd"] = din("exp_wd", [N_EXP, D_EXP, D])
    I["gfin"] = din("gfin", [128, NK])
    I["lam"] = din("lam", [1, 4, 64])
    I["hvec"] = din("hvec", [3, 128])
    K.I = I

    S = {}
    S["mod"] = dscr("s_mod", [2, 128, 6, NK, 2], F32)
    S["qT0"] = dscr("s_qT0", [16, 128, NQ0], BF16)
    S["kT0"] = dscr("s_kT0", [10, 128, NALL], BF16)
    S["v0"] = dscr("s_v0", [10, 128, NALL // 128, 128], BF16)
    S["att0"] = dscr("s_att0", [16, 128, NQ0], BF16)
    S["x1T"] = dscr("s_x1T", [D, NQ0], F32) if start < 5 else din("s_x1T", [D, NQ0])
    S["qT1"] = dscr("s_qT1", [16, 128, NOWN], BF16)
    S["kT1"] = dscr("s_kT1", [10, 128, NQ0], BF16)
    S["v1"] = dscr("s_v1", [10, NQ0, 128], BF16)
    S["att1"] = dscr("s_att1", [16, 128, NOWN], BF16) if start < 7 else din("s_att1", [16, 128, NOWN], BF16)
    S["x2T"] = dscr("s_x2T", [D, NOWN], F32)
    S["x3T"] = dscr("s_x3T", [D, NOWN], F32)
    S["h2"] = dscr("s_h2", [NOWN + 1, D], BF16)
    S["L"] = dscr("s_L", [NSLOT, 1], I32)
    S["Y"] = [dscr(f"s_Y{i}", [NSLOT + 1, D], F32) for i in range(2)]
    K.outT = nc.dram_tensor("outT", [D, NOWN], F32, kind="ExternalOutput").ap()
    K.Lb, K.h2b, K.Yb, K.x2b = Buf(), Buf(), Buf(), Buf()
    K.debug = debug
    W = {}
    for l in (0, 1):
        W[f"w_in{l}"] = nc.dram_tensor(f"wb_in{l}", [D, 4608], BF16, kind="Internal").ap()
        W[f"w_out{l}"] = nc.dram_tensor(f"wb_out{l}", [D, D], BF16, kind="Internal").ap()
    W["ffn_wg"] = nc.dram_tensor("wb_ffn_wg", [D, D_FF], BF16, kind="Internal").ap()
    W["ffn_wu"] = nc.dram_tensor("wb_ffn_wu", [D, D_FF], BF16, kind="Internal").ap()
    W["ffn_wd"] = nc.dram_tensor("wb_ffn_wd", [D_FF, D], BF16, kind="Internal").ap()
    K.W = W
    K.Wb = {k: Buf("W_" + k) for k in W}
    K.S = S

    with ExitStack() as st:
        K.st = st
        K.ps = [st.enter_context(nc.psum_tensor(f"ps{i}", [128, 512], F32)) for i in range(7)]
        K.psb = [Buf(f"ps{i}") for i in range(7)]
        K.pst = st.enter_context(nc.psum_tensor("pst", [128, 1024], BF16))
        K.pstb = [Buf("pst0"), Buf("pst1")]
        K.pstw = [K.pst[:, 0:512], K.ps[6][:].bitcast(BF16)[:, 0:512]]
        K.pstwb = [K.pstb[0], K.psb[6]]
        K.ones = st.enter_context(nc.sbuf_tensor("ones", [128, 128], BF16))
        K.onesf = st.enter_context(nc.sbuf_tensor("onesf", [1, 128], F32))
        K.ident = st.enter_context(nc.sbuf_tensor("ident", [128, 128], BF16))
        K.mod = st.enter_context(nc.sbuf_tensor("modt", [128, 2, 6, NK, 2], F32))
        K.ones32 = st.enter_context(nc.sbuf_tensor("ones32", [128, 128], F32))
        K.cb = Buf("consts")
        K.modb = Buf("mod")
        P.op("vector", lambda e: e.memset(K.ones[:], 1.0), writes=[K.cb])
        P.op("vector", lambda e: e.memset(K.onesf[:], 1.0), writes=[K.cb])
        P.op("vector", lambda e: e.memset(K.ones32[:], 1.0), writes=[K.cb])
        P.op("gpsimd", lambda e: e.memset(K.ident[:], 1.0), writes=[K.cb])
        P.op("gpsimd", lambda e: e.affine_select(out=K.ident[:], in_=K.ident[:], pattern=[[-1, 128]],
                                                 compare_op=ALU.is_equal, fill=0.0, base=0, channel_multiplier=1),
             reads=[K.cb], writes=[K.cb])
        finals = []
        if start < 5:
            cast_weight(K, "w_in0")
        stage_ada(K)
        if debug:
            finals.append(P.dma("sync", S["mod"].rearrange("l p j k v -> p l j k v"), K.mod[:], reads=[K.modb]))
        P.barrier()
        if upto >= 2 and start < 5:
            stage_pre(K, 0)
        if upto >= 3 and start < 5:
            stage_attn0(K)
        if upto >= 4 and start < 5:
            stage_post0(K)
        if upto >= 5 and start < 7:
            stage_pre(K, 1)
        if upto >= 6 and start < 7:
            stage_attn1(K)
        if upto >= 7:
            stage_post1(K)
        if upto >= 8:
            stage_moe(K)
        if upto >= 9:
            finals += stage_final(K)
        P.emit(finals)
    return nc


def cast_weight(K, name):
    P = K.P
    src, dst = K.I[name], K.W[name]
    rows, cols = src.shape
    rstep = 2048 if rows % 2048 == 0 else 1408
    for r0 in range(0, rows, rstep):
        for c0 in range(0, cols, 512):
            c1 = min(cols, c0 + 512)
            P.dma("gpsimd", dst[r0:r0 + rstep, c0:c1], src[r0:r0 + rstep, c0:c1], writes=[K.Wb[name]])


def stage_ada(K):
    nc, P, I = K.nc, K.P, K.I
    with ExitStack() as st:
        T = lambda n, s, d: st.enter_context(nc.sbuf_tensor(n, s, d))
        cf = T("ada_cf", [128, NK, 2], F32)
        sg = T("ada_sg", [128, NK, 2], F32)
        cs = T("ada_cs", [128, NK, 2], BF16)
        wb = [T(f"ada_w{i}", [128, NK, 512], BF16) for i in range(2)]
        wbb = [Buf(), Buf()]
        raw = T("ada_raw", [128, 96, 2], F32)
        bt = T("ada_b", [128, 96], F32)
        gt = T("ada_g", [128, 2, NK], F32)
        b_c, b_raw, b_b, b_g = Buf(), Buf(), Buf(), Buf()
        P.dma("sync", cf[:], I["cT"], writes=[b_c])
        P.op("scalar", lambda e: e.activation(out=sg[:], in_=cf[:], func=AF.Sigmoid), reads=[b_c], writes=[b_raw])
        P.op("vector", lambda e: e.tensor_tensor(out=cs[:], in0=cf[:], in1=sg[:], op=ALU.mult), reads=[b_c, b_raw], writes=[b_c])
        mod = K.mod
        for l in (0, 1):
            P.dma("sync", bt[:], I[f"b_ada{l}"], writes=[b_b])
            P.dma("sync", gt[:, 0, :], I[f"g1_{l}"], writes=[b_g])
            P.dma("sync", gt[:, 1, :], I[f"g2_{l}"], writes=[b_g])
            wv = I[f"w_ada{l}"].rearrange("(k p) n -> p k n", p=128)
            for piece in range(24):
                w = wb[piece % 2]
                wbuf = wbb[piece % 2]
                P.dma("gpsimd", w[:], wv[:, :, piece * 512:(piece + 1) * 512], writes=[wbuf])
                pb = piece % 2
                ps, psb = K.ps[pb], K.psb[pb]
                for mm in range(4):
                    for k in range(NK):
                        P.op("tensor", lambda e, mm=mm, k=k, w=w, ps=ps: e.matmul(ps[:, mm * 2:mm * 2 + 2], w[:, k, mm * 128:(mm + 1) * 128], cs[:, k, :],
                                                                              start=(k == 0), stop=(k == NK - 1)),
                             reads=[wbuf, b_c], writes=[psb])
                P.op("vector", lambda e, piece=piece, ps=ps: e.tensor_copy(out=raw[:, piece * 4:(piece + 1) * 4, :], in_=ps[:, 0:8].rearrange("p (m v) -> p m v", v=2)),
                     reads=[psb], writes=[b_raw])
            for v in range(2):
                P.op("vector", lambda e, v=v: e.tensor_tensor(out=raw[:, :, v], in0=raw[:, :, v], in1=bt[:], op=ALU.add), reads=[b_raw, b_b], writes=[b_raw])
            rv = raw[:].rearrange("p (j k) v -> p j k v", j=6)
            for v in range(2):
                for (slot, src, gi) in ((0, 1, 0), (3, 4, 1)):
                    P.op("vector", lambda e, v=v, slot=slot, src=src, gi=gi, l=l: e.scalar_tensor_tensor(
                        out=mod[:, l, slot, :, v], in0=rv[:, src, :, v], scalar=1.0, in1=gt[:, gi, :], op0=ALU.add, op1=ALU.mult),
                        reads=[b_raw, b_g], writes=[K.modb])
                for (slot, src) in ((1, 0), (2, 2), (4, 3), (5, 5)):
                    P.op("vector", lambda e, v=v, slot=slot, src=src, l=l: e.tensor_copy(out=mod[:, l, slot, :, v], in_=rv[:, src, :, v]),
                         reads=[b_raw], writes=[K.modb])
        P.barrier()


def rms_modulate(K, xt, xb, ht, hb, tt, segs, l, slotA, slotB, tmp, tmpb, sq, sqb, rstd, rstdb, A=None, B=None, extra_reads=()):
    nc, P = K.nc, K.P
    mod = K.mod
    P.op("scalar", lambda e: e.activation(out=sq[:, :, :tt], in_=xt[:, :, :tt], func=AF.Square), reads=[xb], writes=[sqb])
    ps, psb = K.ps[6], K.psb[6]
    for k in range(NK):
        P.op("tensor", lambda e, k=k: e.matmul(ps[:, :tt], K.ones[:], sq[:, k, :tt], start=(k == 0), stop=(k == NK - 1)),
             reads=[sqb, K.cb], writes=[psb])
    P.op("scalar", lambda e: e.activation(out=rstd[:, :tt], in_=ps[:, :tt], func=AF.Sqrt, scale=1.0 / D, bias=EPS), reads=[psb], writes=[rstdb])
    P.op("vector", lambda e: e.reciprocal(out=rstd[:, :tt], in_=rstd[:, :tt]), reads=[rstdb], writes=[rstdb])
    for k in range(NK):
        tm, tmb = tmp[k % 2], tmpb[k % 2]
        P.op("vector", lambda e, k=k, tm=tm: e.tensor_tensor(out=tm[:, :tt], in0=xt[:, k, :tt], in1=rstd[:, :tt], op=ALU.mult),
             reads=[xb, rstdb], writes=[tmb])
        for (c0, c1, v) in segs:
            sA = mod[:, l, slotA, k, v:v + 1] if A is None else A(k, v)
            sB = mod[:, l, slotB, k, v:v + 1] if B is None else B(k, v)
            P.op("scalar", lambda e, k=k, c0=c0, c1=c1, tm=tm, sA=sA, sB=sB: e.activation(out=ht[:, k, c0:c1], in_=tm[:, c0:c1], func=AF.Identity, scale=sA, bias=sB),
                 reads=[tmb, K.modb] + list(extra_reads), writes=[hb])


def rope_pairs(K, eng_a, eng_b, src, dst, cos, sin, ng, npair, t1, t2, t3, t4, rb, wb_, tb):
    P = K.P
    sv = src.rearrange("p (g i t) -> p g i t", g=ng, i=npair, t=2)
    dv = dst.rearrange("p (g i t) -> p g i t", g=ng, i=npair, t=2)
    cb = cos.unsqueeze(1).to_broadcast([128, ng, npair])
    sb = sin.unsqueeze(1).to_broadcast([128, ng, npair])
    v = lambda t: t[:, :ng * npair].rearrange("p (g i) -> p g i", g=ng)
    P.op(eng_a, lambda e: e.tensor_tensor(out=v(t1), in0=sv[:, :, :, 0], in1=cb, op=ALU.mult), reads=rb, writes=[tb[0]])
    P.op(eng_a, lambda e: e.tensor_tensor(out=v(t2), in0=sv[:, :, :, 1], in1=sb, op=ALU.mult), reads=rb, writes=[tb[1]])
    P.op(eng_a, lambda e: e.tensor_tensor(out=dv[:, :, :, 0], in0=v(t1), in1=v(t2), op=ALU.subtract), reads=[tb[0], tb[1]], writes=wb_)
    P.op(eng_b, lambda e: e.tensor_tensor(out=v(t3), in0=sv[:, :, :, 0], in1=sb, op=ALU.mult), reads=rb, writes=[tb[2]])
    P.op(eng_b, lambda e: e.tensor_tensor(out=v(t4), in0=sv[:, :, :, 1], in1=cb, op=ALU.mult), reads=rb, writes=[tb[3]])
    P.op(eng_b, lambda e: e.tensor_tensor(out=dv[:, :, :, 1], in0=v(t3), in1=v(t4), op=ALU.add), reads=[tb[2], tb[3]], writes=wb_)


PRE_CFG = {
    0: {0: ("ropeA", "q", 0, None, None), 1: ("ropeA", "q", 4, None, None), 2: ("ropeB", "q", 8, 0, None), 3: ("ropeB", "q", 12, 0, None),
        4: ("ropeA", "k", 0, None, None), 5: ("ropeA", "k", 4, None, None), 6: ("v", None, None, None, 0), 7: ("v", None, None, None, 4),
        8: ("split", "k", 8, 1, 8)},
    1: {0: ("ropeB", "q", 0, None, None), 1: ("ropeB", "q", 4, None, None), 2: ("plain", "q", 8, None, None), 3: ("plain", "q", 12, None, None),
        4: ("split", "k", 0, None, 0), 5: ("plain", "k", 2, None, None), 6: ("plain", "k", 6, None, None), 7: ("v", None, None, None, 2),
        8: ("v", None, None, None, 6)},
}


def stage_pre(K, l):
    nc, P, I, S = K.nc, K.P, K.I, K.S
    cfg = PRE_CFG[l]
    tiles = []
    if l == 0:
        for t in range(4):
            tiles.append((t * 512, 512, True, [(0, 512, 0)], t * 512, t * 512, t * 512))
        tiles.append((2048, 256, True, [(0, 256, 0)], 2048, 2048, 2048))
        for t in range(3):
            c0 = 2304 + t * 512
            tiles.append((c0, 512, False, [(0, 512, 0)], c0, None, c0))
        tiles.append((2304 + 1536, 256, False, [(0, 256, 0)], 2304 + 1536, None, 2304 + 1536))
        tiles.append((SEQ, 256, True, [(0, 256, 1)], SEQ, NOWN + NHALO, SEQ))
        xv = I["xT"].rearrange("(k p) t -> p k t", p=128)
        qd, kd = S["qT0"], S["kT0"]
    else:
        for t in range(4):
            tiles.append((t * 512, 512, True, [(0, 512, 0)], t * 512, t * 512, t * 512))
        tiles.append((2048, 256, False, [(0, 256, 0)], 2048, None, 2048))
        tiles.append((2304, 256, False, [(0, 256, 1)], SEQ, None, 2304))
        xv = S["x1T"].rearrange("(k p) t -> p k t", p=128)
        qd, kd = S["qT1"], S["kT1"]
    wv = K.W[f"w_in{l}"].rearrange("(k p) n -> p k n", p=128)
    wvb = K.Wb[f"w_in{l}"]
    with ExitStack() as st:
        T = lambda n, s, d: st.enter_context(nc.sbuf_tensor(f"{n}_L{l}", s, d))
        xt = T("pre_x", [128, NK, 512], F32); xb = Buf()
        tmp = [T(f"pre_tmp{i}", [128, 512], F32) for i in range(2)]; tmpb = [Buf(), Buf()]
        sq = T("pre_sq", [128, NK, 512], BF16); sqb = Buf()
        ht = T("pre_h", [128, NK, 512], BF16); hb = Buf()
        rstd = T("pre_rstd", [128, 512], F32); rstdb = Buf()
        wt = [T(f"pre_w{i}", [128, NK, 512], BF16) for i in range(2)]; wtb = [Buf(), Buf()]
        ropet = T("pre_rope", [128, 4, 192], F32); ropeb = Buf()
        xs = [T(f"pre_xs{i}", [128, 512], F32) for i in range(2)]; xsb = [Buf(), Buf()]
        xn = [T(f"pre_xn{i}", [128, 512], F32) for i in range(2)]; xnb = [Buf(), Buf()]
        ob = [T(f"pre_ob{i}", [128, 512], BF16) for i in range(2)]; obb = [Buf(), Buf()]
        tt_ = [T(f"pre_t{i}", [128, 256], F32) for i in range(4)]; ttb = [Buf() for _ in range(4)]
        ss = T("pre_ss", [128, 4], F32); ssb = Buf()
        junk = T("pre_junk", [128, 128], F32); junkb = Buf()
        gq = T("pre_gq", [128, 2, 128], F32); gqb = Buf()
        qT = T("pre_qT", [128, 16, 512], BF16); qTb = Buf()
        kT = T("pre_kT", [128, 10, 512], BF16); kTb = Buf()
        vt = T("pre_v", [128, 10, 4, 128], BF16); vtb = Buf()
        if l == 0:
            P.dma("sync", gq[:, 0, :], I["hvec"][1:2, :].to_broadcast([128, 128]), writes=[gqb])
            P.dma("sync", gq[:, 1, :], I["hvec"][2:3, :].to_broadcast([128, 128]), writes=[gqb])
        cnt = {"w": 0, "e": 0}

        def epilogue(n, s, ps, psb):
            kind, dstk, h0, gi, vh0 = cfg[n]
            ei = cnt["e"]
            cnt["e"] += 1
            x_s, x_sb = xs[ei % 2], xsb[ei % 2]
            x_n, x_nb = xn[ei % 2], xnb[ei % 2]
            o_b, o_bb = ob[ei % 2], obb[ei % 2]
            pstv = K.pstw[ei % 2]
            pstb = K.pstwb[ei % 2]
            cosA, sinA = ropet[:, s, 0:32], ropet[:, s, 32:64]
            cosB, sinB = ropet[:, s, 64:128], ropet[:, s, 128:192]
            if kind == "v":
                P.op("scalar", lambda e: e.copy(out=vt[:, vh0:vh0 + 4, s, :], in_=ps[:].rearrange("p (h d) -> p h d", h=4)), reads=[psb], writes=[vtb])
                return None
            P.op("scalar", lambda e: e.copy(out=x_s[:], in_=ps[:]), reads=[psb], writes=[x_sb])
            nh = 4
            if kind == "split":
                nh = 2
                P.op("vector", lambda e: e.tensor_copy(out=vt[:, vh0:vh0 + 2, s, :], in_=x_s[:, 256:512].rearrange("p (h d) -> p h d", h=2)), reads=[x_sb], writes=[vtb])
                kind = "ropeB"
            src, srcb = x_s, x_sb
            if gi is not None:
                P.op("vector", lambda e: e.memset(ss[:], 0.0), writes=[ssb])
                for h in range(nh):
                    P.op("scalar", lambda e, h=h: e.activation(out=junk[:], in_=x_s[:, h * 128:(h + 1) * 128], func=AF.Square, accum_out=ss[:, h:h + 1]),
                         reads=[x_sb], writes=[junkb, ssb])
                P.op("scalar", lambda e: e.activation(out=ss[:, :nh], in_=ss[:, :nh], func=AF.Sqrt, scale=1.0 / 128, bias=EPS), reads=[ssb], writes=[ssb])
                P.op("vector", lambda e: e.reciprocal(out=ss[:, :nh], in_=ss[:, :nh]), reads=[ssb], writes=[ssb])
                xv3 = x_s[:, :nh * 128].rearrange("p (h d) -> p h d", h=nh)
                xn3 = x_n[:, :nh * 128].rearrange("p (h d) -> p h d", h=nh)
                P.op("vector", lambda e: e.tensor_tensor(out=xn3, in0=xv3, in1=ss[:, :nh].unsqueeze(2).to_broadcast([128, nh, 128]), op=ALU.mult),
                     reads=[x_sb, ssb], writes=[x_nb])
                P.op("gpsimd", lambda e: e.tensor_tensor(out=xn3, in0=xn3, in1=gq[:, gi, :].unsqueeze(1).to_broadcast([128, nh, 128]), op=ALU.mult),
                     reads=[x_nb, gqb], writes=[x_nb])
                src, srcb = x_n, x_nb
            if kind == "ropeA":
                rope_pairs(K, "vector", "gpsimd", src[:], o_b[:], cosA, sinA, 8, 32, tt_[0], tt_[1], tt_[2], tt_[3], [srcb, ropeb], [o_bb], ttb)
                ntr = 4
            elif kind == "ropeB":
                rope_pairs(K, "vector", "gpsimd", src[:, :nh * 128], o_b[:, :nh * 128], cosB, sinB, nh, 64, tt_[0], tt_[1], tt_[2], tt_[3], [srcb, ropeb], [o_bb], ttb)
                ntr = nh
            else:
                P.op("vector", lambda e: e.tensor_copy(out=o_b[:], in_=src[:]), reads=[srcb], writes=[o_bb])
                ntr = 4
            dst, dstb = (qT, qTb) if dstk == "q" else (kT, kTb)

            def part2():
                for h in range(ntr):
                    P.op("tensor", lambda e, h=h: e.transpose(pstv[:, h * 128:(h + 1) * 128], o_b[:, h * 128:(h + 1) * 128], K.ident[:]),
                         reads=[o_bb, K.cb], writes=[pstb])
                P.op("vector", lambda e: e.tensor_copy(out=dst[:, h0:h0 + ntr, s * 128:(s + 1) * 128], in_=pstv[:, :ntr * 128].rearrange("p (h t) -> p h t", h=ntr)),
                     reads=[pstb], writes=[dstb])
            return part2

        def do_tile(t0, tt, need_q, segs, r0, qcol, kcol):
            nsub = tt // 128
            P.dma("sync", xt[:, :, :tt], xv[:, :, t0:t0 + tt], writes=[xb])
            P.dma("sync", ropet[:, :nsub, :], I["rope"][r0:r0 + tt, :].rearrange("(s p) c -> p s c", p=128), writes=[ropeb])
            rms_modulate(K, xt, xb, ht, hb, tt, segs, l, 0, 1, tmp, tmpb, sq, sqb, rstd, rstdb)
            pending = None
            for n in range(9):
                if not need_q and cfg[n][1] == "q":
                    continue
                w, wbuf = wt[cnt["w"] % 2], wtb[cnt["w"] % 2]
                cnt["w"] += 1
                P.dma("sync", w[:], wv[:, :, n * 512:(n + 1) * 512], reads=[wvb], writes=[wbuf])
                for s in range(nsub):
                    bi = cnt["e"] % 4
                    ps, psb = K.ps[bi], K.psb[bi]
                    for k in range(NK):
                        P.op("tensor", lambda e, k=k, s=s, w=w, ps=ps: e.matmul(ps[:], ht[:, k, s * 128:(s + 1) * 128], w[:, k, :], start=(k == 0), stop=(k == NK - 1)),
                             reads=[hb, wbuf], writes=[psb])
                    if pending is not None:
                        pending()
                    pending = epilogue(n, s, ps, psb)
            if pending is not None:
                pending()
            if need_q:
                P.dma("sync", qd[:, :, qcol:qcol + tt].rearrange("h p t -> p h t"), qT[:, :, :tt], reads=[qTb])
            P.dma("sync", kd[:, :, kcol:kcol + tt].rearrange("h p t -> p h t"), kT[:, :, :tt], reads=[kTb])
            if l == 0:
                P.dma("sync", S["v0"][:, :, kcol // 128:kcol // 128 + nsub, :].rearrange("h p s d -> p h s d"), vt[:, :, :nsub, :], reads=[vtb])
            else:
                for s in range(nsub):
                    P.dma("sync", S["v1"][:, kcol + s * 128:kcol + (s + 1) * 128, :].rearrange("h p d -> p h d"), vt[:, :, s, :], reads=[vtb])

        for tl in tiles:
            do_tile(*tl)
        P.barrier()


def stage_attn0(K):
    nc, P, I, S = K.nc, K.P, K.I, K.S
    ATT_SCALE = 128 ** -0.5
    A_SCALE = 64 ** -0.5
    with ExitStack() as st:
        T = lambda n, s, d: st.enter_context(nc.sbuf_tensor(n, s, d))
        kt = [T(f"at_k{i}", [128, NALL], BF16) for i in range(2)]; ktb = [Buf(), Buf()]
        vt = [T(f"at_v{i}", [128, NALL // 128, 128], BF16) for i in range(2)]; vtb = [Buf(), Buf()]
        qt = [T(f"at_q{i}", [128, 512], BF16) for i in range(2)]; qtb = [Buf(), Buf()]
        pt = [T(f"at_p{i}", [128, 512], BF16) for i in range(4)]; ptb = [Buf() for _ in range(4)]
        rec = [T(f"at_rec{i}", [128, 512], F32) for i in range(2)]; recb = [Buf(), Buf()]
        o0 = T("at_o0", [128, 512], F32); o0b = Buf()
        o1 = T("at_o1", [128, 512], F32); o1b = Buf()
        osq = T("at_osq", [128, 512], BF16); osqb = Buf()
        ot = [T(f"at_ot{i}", [128, 512], BF16) for i in range(2)]; otb = [Buf(), Buf()]
        accs = [[T(f"at_acc{c}{g}", [128, 512], F32) for g in range(2)] for c in range(2)]
        accb = [[Buf(), Buf()] for c in range(2)]
        lt = T("at_lt", [1, 4, 64], F32); ltb = Buf()
        lp = T("at_lp", [1, 2, 64], F32)
        l2 = T("at_l2", [1, 2], F32)
        l1 = T("at_l1", [1, 1], F32)
        nlam = T("at_nlam", [128, 1], F32); nlamb = Buf()
        sg = T("at_sg", [128, 1], F32); sgb = Buf()
        P.dma("sync", lt[:], I["lam"], writes=[ltb])
        P.op("vector", lambda e: e.tensor_tensor(out=lp[:], in0=lt[:, 0::2, :], in1=lt[:, 1::2, :], op=ALU.mult), reads=[ltb], writes=[ltb])
        P.op("vector", lambda e: e.tensor_reduce(out=l2[:], in_=lp[:], axis=AX.X, op=ALU.add), reads=[ltb], writes=[ltb])
        P.op("scalar", lambda e: e.activation(out=l2[:], in_=l2[:], func=AF.Exp), reads=[ltb], writes=[ltb])
        P.op("vector", lambda e: e.scalar_tensor_tensor(out=l1[:], in0=l2[:, 1:2], scalar=-LAMBDA_INIT_L0, in1=l2[:, 0:1], op0=ALU.add, op1=ALU.subtract), reads=[ltb], writes=[ltb])
        P.op("tensor", lambda e: e.matmul(K.ps[0][:, 0:1], K.onesf[:], l1[:], start=True, stop=True), reads=[ltb, K.cb], writes=[K.psb[0]])
        P.op("vector", lambda e: e.tensor_copy(out=nlam[:], in_=K.ps[0][:, 0:1]), reads=[K.psb[0]], writes=[nlamb])
        P.dma("sync", sg[:], I["hvec"][0:1, :].rearrange("o d -> d o"), writes=[sgb])
        P.op("vector", lambda e: e.tensor_scalar(out=sg[:], in0=sg[:], scalar1=1.0 - LAMBDA_INIT_L0, scalar2=None, op0=ALU.mult), reads=[sgb], writes=[sgb])

        for nm in ("w_out0", "ffn_wg", "ffn_wu", "ffn_wd", "w_in1", "w_out1"):
            cast_weight(K, nm)
        qtiles = [(t * 512, 512, list(range(34))) for t in range(4)] + [(2048, 256, list(range(34))), (2304, 256, [32, 33])]
        cnt = {"q": 0, "s": 0, "p": 0, "o": 0}
        def do_tile(kk, kkb, vv, vvb, qh, q0, tt, chunks, is_a, ncomp, scale):
            q_, q_b = qt[cnt["q"] % 2], qtb[cnt["q"] % 2]
            cnt["q"] += 1
            P.dma("sync", q_[:, :tt], S["qT0"][qh, :, q0:q0 + tt], writes=[q_b])
            jobs = [(kc, c) for kc in chunks for c in range(ncomp)]
            sbank = {}
            used = set()

            def issue_s(j):
                kc, c = jobs[j]
                bi = cnt["s"] % 3
                cnt["s"] += 1
                sbank[j] = bi
                if is_a:
                    lo, hi = c * 64, (c + 1) * 64
                else:
                    lo, hi = 0, 128
                P.op("tensor", lambda e, kc=kc, lo=lo, hi=hi, bi=bi: e.matmul(K.ps[bi][:, :tt], kk[lo:hi, kc * 128:(kc + 1) * 128], q_[lo:hi, :tt], start=True, stop=True),
                     reads=[kkb, q_b], writes=[K.psb[bi]])
            LA = 2
            for j in range(min(LA, len(jobs))):
                issue_s(j)
            for j, (kc, c) in enumerate(jobs):
                bi = sbank[j]
                p_, p_b = pt[cnt["p"] % 4], ptb[cnt["p"] % 4]
                cnt["p"] += 1
                P.op("scalar", lambda e, bi=bi, p_=p_: e.activation(out=p_[:, :tt], in_=K.ps[bi][:, :tt], func=AF.Exp, scale=scale), reads=[K.psb[bi]], writes=[p_b])
                if j + LA < len(jobs):
                    issue_s(j + LA)
                first, last = (kc == chunks[0]), (kc == chunks[-1])
                P.op("tensor", lambda e, kc=kc, c=c, p_=p_, first=first, last=last: e.matmul(K.ps[3 + c][:, :tt], vv[:, kc, :], p_[:, :tt], start=first, stop=last),
                     reads=[vvb, p_b], writes=[K.psb[3 + c]])
                g = 1 if (j % 3 == 2) else 0
                eng = "gpsimd" if g else "vector"
                a_, a_b = accs[c][g], accb[c][g]
                if (c, g) not in used:
                    used.add((c, g))
                    P.op(eng, lambda e, a_=a_, p_=p_: e.tensor_copy(out=a_[:, :tt], in_=p_[:, :tt]), reads=[p_b], writes=[a_b])
                else:
                    P.op(eng, lambda e, a_=a_, p_=p_: e.tensor_tensor(out=a_[:, :tt], in0=a_[:, :tt], in1=p_[:, :tt], op=ALU.add), reads=[p_b, a_b], writes=[a_b])
            for c in range(ncomp):
                gs = [g for g in range(2) if (c, g) in used]
                for n_, g in enumerate(gs):
                    P.op("tensor", lambda e, c=c, g=g, n_=n_, gs=gs: e.matmul(K.ps[5 + c][:, :tt], K.ones32[:], accs[c][g][:, :tt], start=(n_ == 0), stop=(n_ == len(gs) - 1)),
                         reads=[K.cb, accb[c][g]], writes=[K.psb[5 + c]])
            o_, o_b = ot[cnt["o"] % 2], otb[cnt["o"] % 2]
            cnt["o"] += 1
            for c in range(ncomp):
                P.op("vector", lambda e, c=c: e.reciprocal(out=rec[c][:, :tt], in_=K.ps[5 + c][:, :tt]), reads=[K.psb[5 + c]], writes=[recb[c]])
            if not is_a:
                P.op("vector", lambda e, o_=o_: e.tensor_tensor(out=o_[:, :tt], in0=K.ps[3][:, :tt], in1=rec[0][:, :tt], op=ALU.mult), reads=[K.psb[3], recb[0]], writes=[o_b])
            else:
                P.op("vector", lambda e: e.tensor_tensor(out=o0[:, :tt], in0=K.ps[3][:, :tt], in1=rec[0][:, :tt], op=ALU.mult), reads=[K.psb[3], recb[0]], writes=[o0b])
                P.op("vector", lambda e: e.tensor_tensor(out=o1[:, :tt], in0=K.ps[4][:, :tt], in1=rec[1][:, :tt], op=ALU.mult), reads=[K.psb[4], recb[1]], writes=[o1b])
                P.op("vector", lambda e: e.scalar_tensor_tensor(out=o0[:, :tt], in0=o1[:, :tt], scalar=nlam[:, 0:1], in1=o0[:, :tt], op0=ALU.mult, op1=ALU.add),
                     reads=[o0b, o1b, nlamb], writes=[o0b])
                P.op("scalar", lambda e: e.activation(out=osq[:, :tt], in_=o0[:, :tt], func=AF.Square), reads=[o0b], writes=[osqb])
                bi = cnt["s"] % 3
                cnt["s"] += 1
                P.op("tensor", lambda e, bi=bi: e.matmul(K.ps[bi][:, :tt], K.ones[:], osq[:, :tt], start=True, stop=True), reads=[osqb, K.cb], writes=[K.psb[bi]])
                P.op("scalar", lambda e, bi=bi: e.activation(out=rec[0][:, :tt], in_=K.ps[bi][:, :tt], func=AF.Sqrt, scale=1.0 / 128, bias=EPS), reads=[K.psb[bi]], writes=[recb[0]])
                P.op("vector", lambda e: e.reciprocal(out=rec[0][:, :tt], in_=rec[0][:, :tt]), reads=[recb[0]], writes=[recb[0]])
                P.op("vector", lambda e, o_=o_: e.scalar_tensor_tensor(out=o_[:, :tt], in0=o0[:, :tt], scalar=sg[:, 0:1], in1=rec[0][:, :tt], op0=ALU.mult, op1=ALU.mult),
                     reads=[o0b, sgb, recb[0]], writes=[o_b])
            P.dma("sync", S["att0"][qh, :, q0:q0 + tt], o_[:, :tt], reads=[o_b])

        for kvh in range(10):
            kk, kkb = kt[kvh % 2], ktb[kvh % 2]
            vv, vvb = vt[kvh % 2], vtb[kvh % 2]
            P.dma("sync", kk[:], S["kT0"][kvh], writes=[kkb])
            P.dma("sync", vv[:], S["v0"][kvh], writes=[vvb])
            is_a = kvh < 8
            qheads = [kvh] if is_a else list(range(8 + (kvh - 8) * 4, 12 + (kvh - 8) * 4))
            ncomp = 2 if is_a else 1
            scale = A_SCALE if is_a else ATT_SCALE
            for qh in qheads:
                for (q0, tt, chunks) in qtiles:
                    do_tile(kk, kkb, vv, vvb, qh, q0, tt, chunks, is_a, ncomp, scale)
        P.barrier()


def stage_post0(K):
    nc, P, I, S = K.nc, K.P, K.I, K.S
    l = 0
    NJ = D_FF // 128
    xv = I["xT"].rearrange("(k p) t -> p k t", p=128)
    wov = K.W["w_out0"].rearrange("(k p) n -> p k n", p=128)
    wgv = K.W["ffn_wg"].rearrange("(k p) n -> p k n", p=128)
    wuv = K.W["ffn_wu"].rearrange("(k p) n -> p k n", p=128)
    wdv = K.W["ffn_wd"].rearrange("(j p) n -> p j n", p=128)
    x1v = S["x1T"].rearrange("(k p) t -> p k t", p=128)
    with ExitStack() as st:
        T = lambda n, s, d: st.enter_context(nc.sbuf_tensor(n, s, d))
        big = T("po_big", [128, NJ, 512], BF16); bigb = Buf()
        xt = T("po_x", [128, NK, 512], F32); xb = Buf()
        tmp = [T(f"po_tmp{i}", [128, 512], F32) for i in range(2)]; tmpb = [Buf(), Buf()]
        ht = T("po_h", [128, NK, 512], BF16); hb = Buf()
        rstd = T("po_rstd", [128, 512], F32); rstdb = Buf()
        wt = [T(f"po_w{i}", [128, NK, 512], BF16) for i in range(2)]; wtb = [Buf(), Buf()]
        wd = [T(f"po_wd{i}", [128, NJ, 256], BF16) for i in range(2)]; wdb = [Buf(), Buf()]
        sl = [T(f"po_sl{i}", [128, 512], F32) for i in range(2)]; slb = [Buf(), Buf()]
        att = big[:, 0:NK, :]
        sq = big[:, NK:2 * NK, :]
        cnt = {"w": 0, "wd": 0, "ps": 0, "sl": 0}

        def do_tile(q0, xsrc, segs):
            P.dma("sync", att, S["att0"][:, :, q0:q0 + 512].rearrange("h p t -> p h t"), writes=[bigb])
            for (c0, c1, s0) in xsrc:
                P.dma("sync", xt[:, :, c0:c1], xv[:, :, s0:s0 + (c1 - c0)], writes=[xb])
            for pc in range(4):
                w, wbuf = wt[cnt["w"] % 2], wtb[cnt["w"] % 2]
                cnt["w"] += 1
                P.dma("sync", w[:], wov[:, :, pc * 512:(pc + 1) * 512], reads=[K.Wb["w_out0"]], writes=[wbuf])
                for mm in range(4):
                    m = pc * 4 + mm
                    bi = cnt["ps"] % 4
                    cnt["ps"] += 1
                    for k in range(NK):
                        P.op("tensor", lambda e, k=k, mm=mm, w=w, bi=bi: e.matmul(K.ps[bi][:], w[:, k, mm * 128:(mm + 1) * 128], att[:, k, :], start=(k == 0), stop=(k == NK - 1)),
                             reads=[wbuf, bigb], writes=[K.psb[bi]])
                    for (c0, c1, v) in segs:
                        P.op("vector", lambda e, m=m, bi=bi, c0=c0, c1=c1, v=v: e.scalar_tensor_tensor(out=xt[:, m, c0:c1], in0=K.ps[bi][:, c0:c1], scalar=K.mod[:, l, 2, m, v:v + 1],
                                                                                                  in1=xt[:, m, c0:c1], op0=ALU.mult, op1=ALU.add),
                             reads=[K.psb[bi], xb, K.modb], writes=[xb])
            if CUT == 1:
                P.dma("sync", x1v[:, :, q0:q0 + 512], xt[:], reads=[xb]); return
            rms_modulate(K, xt, xb, ht, hb, 512, segs, l, 3, 4, tmp, tmpb, sq, bigb, rstd, rstdb)
            if CUT == 2:
                P.dma("sync", x1v[:, :, q0:q0 + 512], xt[:], reads=[xb]); return
            for jp in range(NJ // 2):
                w, wbuf = wt[cnt["w"] % 2], wtb[cnt["w"] % 2]
                cnt["w"] += 1
                P.dma("sync", w[:, :, 0:256], wgv[:, :, jp * 256:(jp + 1) * 256], reads=[K.Wb["ffn_wg"]], writes=[wbuf])
                P.dma("sync", w[:, :, 256:512], wuv[:, :, jp * 256:(jp + 1) * 256], reads=[K.Wb["ffn_wu"]], writes=[wbuf])
                for jj in range(2):
                    j = jp * 2 + jj
                    bg = (cnt["ps"] % 2) * 2
                    cnt["ps"] += 1
                    for k in range(NK):
                        P.op("tensor", lambda e, k=k, jj=jj, w=w, bg=bg: e.matmul(K.ps[bg][:], w[:, k, jj * 128:(jj + 1) * 128], ht[:, k, :], start=(k == 0), stop=(k == NK - 1)),
                             reads=[wbuf, hb], writes=[K.psb[bg]])
                    for k in range(NK):
                        P.op("tensor", lambda e, k=k, jj=jj, w=w, bg=bg: e.matmul(K.ps[bg + 1][:], w[:, k, 256 + jj * 128:256 + (jj + 1) * 128], ht[:, k, :], start=(k == 0), stop=(k == NK - 1)),
                             reads=[wbuf, hb], writes=[K.psb[bg + 1]])
                    s_, s_b = sl[cnt["sl"] % 2], slb[cnt["sl"] % 2]
                    cnt["sl"] += 1
                    P.op("scalar", lambda e, bg=bg, s_=s_: e.activation(out=s_[:], in_=K.ps[bg][:], func=AF.Silu), reads=[K.psb[bg]], writes=[s_b])
                    P.op("vector", lambda e, bg=bg, s_=s_, j=j: e.tensor_tensor(out=big[:, j, :], in0=s_[:], in1=K.ps[bg + 1][:], op=ALU.mult),
                         reads=[s_b, K.psb[bg + 1]], writes=[bigb])
            if CUT == 3:
                P.dma("sync", x1v[:, :, q0:q0 + 512], xt[:], reads=[xb]); return
            for pc in range(8):
                w, wbuf = wd[cnt["wd"] % 2], wdb[cnt["wd"] % 2]
                cnt["wd"] += 1
                for jq in range(4):
                    P.dma("sync", w[:, jq * 11:(jq + 1) * 11, :], wdv[:, jq * 11:(jq + 1) * 11, pc * 256:(pc + 1) * 256], reads=[K.Wb["ffn_wd"]], writes=[wbuf])
                for mm in range(2):
                    m = pc * 2 + mm
                    bi = 4 + cnt["ps"] % 2
                    cnt["ps"] += 1
                    for j in range(NJ):
                        P.op("tensor", lambda e, j=j, mm=mm, w=w, bi=bi: e.matmul(K.ps[bi][:], w[:, j, mm * 128:(mm + 1) * 128], big[:, j, :], start=(j == 0), stop=(j == NJ - 1)),
                             reads=[wbuf, bigb], writes=[K.psb[bi]])
                    for (c0, c1, v) in segs:
                        P.op("vector", lambda e, m=m, bi=bi, c0=c0, c1=c1, v=v: e.scalar_tensor_tensor(out=xt[:, m, c0:c1], in0=K.ps[bi][:, c0:c1], scalar=K.mod[:, l, 5, m, v:v + 1],
                                                                                                  in1=xt[:, m, c0:c1], op0=ALU.mult, op1=ALU.add),
                             reads=[K.psb[bi], xb, K.modb], writes=[xb])
            P.dma("sync", x1v[:, :, q0:q0 + 512], xt[:], reads=[xb])

        for t in range(4):
            do_tile(t * 512, [(0, 512, t * 512)], [(0, 512, 0)])
        do_tile(2048, [(0, 256, 2048), (256, 512, SEQ)], [(0, 256, 0), (256, 512, 1)])
        P.barrier()


def stage_attn1(K):
    nc, P, I, S = K.nc, K.P, K.I, K.S
    ATT_SCALE = 128 ** -0.5
    with ExitStack() as st:
        T = lambda n, s, d: st.enter_context(nc.sbuf_tensor(n, s, d))
        kt = [T(f"a1_k{i}", [128, NQ0], BF16) for i in range(2)]; ktb = [Buf(), Buf()]
        vc = [T(f"a1_vc{i}", [128, 20, 128], BF16) for i in range(2)]; vcb = [Buf(), Buf()]
        vd = [T(f"a1_vd{i}", [64, 40, 128], BF16) for i in range(2)]; vdb = [Buf(), Buf()]
        qt = [T(f"a1_q{i}", [128, NOWN], BF16) for i in range(2)]; qtb = [Buf(), Buf()]
        pt = [T(f"a1_p{i}", [128, 512], BF16) for i in range(4)]; ptb = [Buf() for _ in range(4)]
        mk = T("a1_mask", [128, 8, 512], BF16); mkb = Buf()
        rec = T("a1_rec", [128, 512], F32); recb = Buf()
        ot = [T(f"a1_ot{i}", [128, 512], BF16) for i in range(2)]; otb = [Buf(), Buf()]
        sk = T("a1_sk", [1, 8], F32); skb = Buf()
        es = T("a1_es", [128, 8], F32); esb = Buf()
        bg = [T(f"a1_bg{i}", [64, 8, 64], F32) for i in range(2)]; bgb = [Buf(), Buf()]
        be = [T(f"a1_be{i}", [64, 12, 64], F32) for i in range(2)]; beb = [Buf(), Buf()]
        tA = [T(f"a1_tA{i}", [64, 512], F32) for i in range(2)]; tAb = [Buf(), Buf()]
        tB = [T(f"a1_tB{i}", [64, 256], F32) for i in range(2)]; tBb = [Buf(), Buf()]
        pA = [T(f"a1_pA{i}", [64, 512], BF16) for i in range(2)]; pAb = [Buf(), Buf()]
        pB = [T(f"a1_pB{i}", [64, 512], BF16) for i in range(2)]; pBb = [Buf(), Buf()]
        P.dma("gpsimd", mk[:], I["maskC"].rearrange("m p q -> p m q"), writes=[mkb])
        P.dma("sync", sk[:], I["sinks"], writes=[skb])
        P.op("scalar", lambda e: e.activation(out=sk[:], in_=sk[:], func=AF.Exp), reads=[skb], writes=[skb])
        P.op("tensor", lambda e: e.matmul(K.ps[0][:, 0:8], K.onesf[:], sk[:], start=True, stop=True), reads=[skb, K.cb], writes=[K.psb[0]])
        P.op("vector", lambda e: e.tensor_copy(out=es[:], in_=K.ps[0][:, 0:8]), reads=[K.psb[0]], writes=[esb])
        cnt = {"q": 0, "s": 0, "p": 0, "o": 0, "kv": 0}

        def c_tile(kk, kkb, vv, vvb, q_, q_b, qh, t):
            n0 = 4 * t
            chunks = []
            for r in range(-1, 5):
                kb = n0 + r
                if kb < 0:
                    chunks.append((2176, 17, 6))
                elif kb > 15:
                    chunks.append((2048, 16, 7))
                else:
                    chunks.append((kb * 128, kb, r + 1))
            chunks += [(2304, 18, None), (2432, 19, None)]
            sbank = {}

            def issue_s(j):
                col = chunks[j][0]
                bi = cnt["s"] % 3
                cnt["s"] += 1
                sbank[j] = bi
                P.op("tensor", lambda e: e.matmul(K.ps[bi][:], kk[:, col:col + 128], q_[:, t * 512:(t + 1) * 512], start=True, stop=True),
                     reads=[kkb, q_b], writes=[K.psb[bi]])
            LA = 2
            for j in range(LA):
                issue_s(j)
            for j, (col, vch, mi) in enumerate(chunks):
                bi = sbank[j]
                p_, p_b = pt[cnt["p"] % 4], ptb[cnt["p"] % 4]
                cnt["p"] += 1
                P.op("scalar", lambda e, bi=bi, p_=p_: e.activation(out=p_[:], in_=K.ps[bi][:], func=AF.Exp, scale=ATT_SCALE), reads=[K.psb[bi]], writes=[p_b])
                if mi is not None:
                    P.op("vector", lambda e, p_=p_, mi=mi: e.tensor_tensor(out=p_[:], in0=p_[:], in1=mk[:, mi, :], op=ALU.mult), reads=[p_b, mkb], writes=[p_b])
                if j + LA < len(chunks):
                    issue_s(j + LA)
                first, last = (j == 0), (j == len(chunks) - 1)
                P.op("tensor", lambda e, vch=vch, p_=p_, first=first, last=last: e.matmul(K.ps[3][:], vv[:, vch, :], p_[:], start=first, stop=last),
                     reads=[vvb, p_b], writes=[K.psb[3]])
                P.op("tensor", lambda e, p_=p_, first=first, last=last: e.matmul(K.ps[5][:], K.ones[:], p_[:], start=first, stop=last),
                     reads=[K.cb, p_b], writes=[K.psb[5]])
            o_, o_b = ot[cnt["o"] % 2], otb[cnt["o"] % 2]
            cnt["o"] += 1
            P.op("vector", lambda e: e.tensor_scalar(out=rec[:], in0=K.ps[5][:], scalar1=es[:, qh:qh + 1], scalar2=None, op0=ALU.add), reads=[K.psb[5], esb], writes=[recb])
            P.op("vector", lambda e: e.reciprocal(out=rec[:], in_=rec[:]), reads=[recb], writes=[recb])
            P.op("vector", lambda e: e.tensor_tensor(out=o_[:], in0=K.ps[3][:], in1=rec[:], op=ALU.mult), reads=[K.psb[3], recb], writes=[o_b])
            P.dma("sync", S["att1"][qh, :, t * 512:(t + 1) * 512], o_[:], reads=[o_b])

        for kvh in range(2):
            i = cnt["kv"] % 2
            cnt["kv"] += 1
            P.dma("sync", kt[i][:], S["kT1"][kvh], writes=[ktb[i]])
            P.dma("sync", vc[i][:], S["v1"][kvh].rearrange("(c p) d -> p c d", p=128), writes=[vcb[i]])
            for g in range(4):
                qh = kvh * 4 + g
                qi = cnt["q"] % 2
                cnt["q"] += 1
                P.dma("sync", qt[qi][:], S["qT1"][qh], writes=[qtb[qi]])
                for t in range(4):
                    c_tile(kt[i], ktb[i], vc[i], vcb[i], qt[qi], qtb[qi], qh, t)

        def d_head(h):
            i = cnt["kv"] % 2
            cnt["kv"] += 1
            kk, kkb, vv, vvb = kt[i], ktb[i], vd[i], vdb[i]
            P.dma("sync", kk[:], S["kT1"][2 + h], writes=[kkb])
            vsrc = S["v1"][2 + h].rearrange("(r p) d -> p r d", p=64)
            P.dma("sync", vv[:, 0:20, :], vsrc[:, 0:20, :], writes=[vvb])
            P.dma("sync", vv[:, 20:40, :], vsrc[:, 20:40, :], writes=[vvb])
            qi = cnt["q"] % 2
            cnt["q"] += 1
            q_, q_b = qt[qi], qtb[qi]
            P.dma("sync", q_[:], S["qT1"][8 + h], writes=[q_b])
            bgt, bgtb = bg[h % 2], bgb[h % 2]
            P.dma("sync", bgt[:], I["biasD"][h], writes=[bgtb])
            state = {}

            def row_scores(r):
                edge = (r < 4) or (r > 27)
                if not edge:
                    own = list(range(r - 4, r + 4))
                    bias, biasb = bgt, bgtb
                else:
                    own = list(range(0, 8)) if r < 4 else list(range(24, 32))
                    er = r if r < 4 else r - 24
                    bias, biasb = be[er % 2], beb[er % 2]
                    P.dma("sync", bias[:], I["biasE"][er, h], writes=[biasb])
                sa = cnt["s"] % 2
                cnt["s"] += 1
                qs = q_[:, r * 64:(r + 1) * 64]
                for c, sr in enumerate(own):
                    P.op("tensor", lambda e, c=c, sr=sr: e.matmul(K.ps[sa][0:64, c * 64:(c + 1) * 64], kk[:, sr * 64:(sr + 1) * 64], qs, start=True, stop=True),
                         reads=[kkb, q_b], writes=[K.psb[sa]])
                extra = ([32, 33, 34, 35] if edge else []) + [36, 37, 38, 39]
                off = 0 if edge else 4
                for c, sr in enumerate(extra):
                    cc = c + off
                    P.op("tensor", lambda e, cc=cc, sr=sr: e.matmul(K.ps[2][0:64, cc * 64:(cc + 1) * 64], kk[:, sr * 64:(sr + 1) * 64], qs, start=True, stop=True),
                         reads=[kkb, q_b], writes=[K.psb[2]])
                ta, tab = tA[r % 2], tAb[r % 2]
                tb, tbb = tB[r % 2], tBb[r % 2]
                pa, pab = pA[r % 2], pAb[r % 2]
                pb, pbb = pB[r % 2], pBb[r % 2]
                P.op("vector", lambda e: e.scalar_tensor_tensor(out=ta[:], in0=K.ps[sa][0:64, :], scalar=ATT_SCALE, in1=bias[:, 0:8, :].rearrange("p c q -> p (c q)"),
                                                                op0=ALU.mult, op1=ALU.add), reads=[K.psb[sa], biasb], writes=[tab])
                P.op("scalar", lambda e: e.activation(out=pa[:], in_=ta[:], func=AF.Exp), reads=[tab], writes=[pab])
                if edge:
                    P.op("vector", lambda e: e.scalar_tensor_tensor(out=tb[:], in0=K.ps[2][0:64, 0:256], scalar=ATT_SCALE, in1=bias[:, 8:12, :].rearrange("p c q -> p (c q)"),
                                                                    op0=ALU.mult, op1=ALU.add), reads=[K.psb[2], biasb], writes=[tbb])
                    P.op("scalar", lambda e: e.activation(out=pb[:, 0:256], in_=tb[:], func=AF.Exp), reads=[tbb], writes=[pbb])
                P.op("scalar", lambda e: e.activation(out=pb[:, 256:512], in_=K.ps[2][0:64, 256:512], func=AF.Exp, scale=ATT_SCALE), reads=[K.psb[2]], writes=[pbb])
                state[r] = (own, extra, off, pa, pab, pb, pbb)

            def row_pv(r):
                own, extra, off, pa, pab, pb, pbb = state.pop(r)
                grp = r // 8
                po, pob = K.ps[3 + grp % 2], K.psb[3 + grp % 2]
                pss, pssb = K.ps[5 + grp % 2], K.psb[5 + grp % 2]
                cs = slice((r % 8) * 64, (r % 8) * 64 + 64)
                items = [(sr, pa, pab, c) for c, sr in enumerate(own)] + [(sr, pb, pbb, c + off) for c, sr in enumerate(extra)]
                for n_, (sr, pp, ppb, c) in enumerate(items):
                    first, last = (n_ == 0), (n_ == len(items) - 1)
                    P.op("tensor", lambda e, sr=sr, pp=pp, c=c, first=first, last=last: e.matmul(po[:, cs], vv[:, sr, :], pp[:, c * 64:(c + 1) * 64], start=first, stop=last),
                         reads=[vvb, ppb], writes=[pob])
                    P.op("tensor", lambda e, pp=pp, c=c, first=first, last=last: e.matmul(pss[:, cs], K.ones[0:64, :], pp[:, c * 64:(c + 1) * 64], start=first, stop=last),
                         reads=[K.cb, ppb], writes=[pssb])
                if r % 8 == 7:
                    o_, o_b = ot[cnt["o"] % 2], otb[cnt["o"] % 2]
                    cnt["o"] += 1
                    P.op("vector", lambda e: e.reciprocal(out=rec[:], in_=pss[:]), reads=[pssb], writes=[recb])
                    P.op("vector", lambda e: e.tensor_tensor(out=o_[:], in0=po[:], in1=rec[:], op=ALU.mult), reads=[pob, recb], writes=[o_b])
                    P.dma("sync", S["att1"][8 + h, :, grp * 512:(grp + 1) * 512], o_[:], reads=[o_b])

            row_scores(0)
            for r in range(32):
                if r + 1 < 32:
                    row_scores(r + 1)
                row_pv(r)

        for h in range(8):
            d_head(h)
        P.barrier()


CAP = 1024
NSLOT = N_EXP * CAP
NJH = D_EXP // 128 // 2


def stage_post1(K):
    nc, P, I, S = K.nc, K.P, K.I, K.S
    l = 1
    x1v = S["x1T"].rearrange("(k p) t -> p k t", p=128)
    x2v = S["x2T"].rearrange("(k p) t -> p k t", p=128)
    wov = K.W["w_out1"].rearrange("(k p) n -> p k n", p=128)
    st = K.st
    TP = lambda n, s_, d: st.enter_context(nc.sbuf_tensor(n, s_, d))
    K.gate = TP("gate12", [128, 16, 2], F32); K.gateb = Buf()
    K.sloti = TP("slot12i", [128, 16, 2], I32); K.slotib = Buf()
    with ExitStack() as st2:
        T = lambda n, s_, d: st2.enter_context(nc.sbuf_tensor(n, s_, d))
        att = T("p1_att", [128, NK, 512], BF16); attb = Buf()
        sq = T("p1_sq", [128, NK, 512], BF16); sqb = Buf()
        xt = T("p1_x", [128, NK, 512], F32); xb = Buf()
        tmp = [T(f"p1_tmp{i}", [128, 512], F32) for i in range(2)]; tmpb = [Buf(), Buf()]
        ht = T("p1_h", [128, NK, 512], BF16); hb = Buf()
        rstd = T("p1_rstd", [128, 512], F32); rstdb = Buf()
        wt = [T(f"p1_w{i}", [128, NK, 512], BF16) for i in range(2)]; wtb = [Buf(), Buf()]
        wr = T("p1_wr", [128, NK, 8], BF16); wrb = Buf()
        htok = [T(f"p1_htok{i}", [128, D], BF16) for i in range(2)]; htokb = [Buf(), Buf()]
        U = T("p1_U", [128, 128], BF16); Ub = Buf()
        lg = T("p1_lg", [128, 8], F32); lgb = Buf()
        mx = T("p1_mx", [128, 8], F32); mxb = Buf()
        ind = T("p1_ind", [128, 3, 8], F32); indb = Buf()
        indh = T("p1_indh", [128, 8], BF16); indhb = Buf()
        base = T("p1_base", [128, 8], F32); baseb = Buf()
        pos = T("p1_pos", [128, 8], F32); posb = Buf()
        val = T("p1_val", [128, 8], F32); valb = Buf()
        eoff = T("p1_eoff", [128, 8], F32); eoffb = Buf()
        eoffi = T("p1_eoffi", [128, 8], I32)
        pidi = T("p1_pidi", [128, 1], I32)
        pidf = T("p1_pidf", [128, 1], F32); pidb = Buf()
        tokf = T("p1_tokf", [128, 1], F32)
        toki = [T(f"p1_toki{i}", [128, 1], I32) for i in range(2)]; tokb = [Buf(), Buf()]
        slf = T("p1_slf", [128, 2], F32); slfb = Buf()
        junk8 = T("p1_junk8", [128, 8], F32); junk8b = Buf()
        dl = T("p1_dl", [128, 1], F32); dlb = Buf()
        cfill = T("p1_cfill", [128, NSLOT // 128], I32); cfb = Buf()
        zrow = T("p1_zrow", [1, D], BF16); zrb = Buf()
        zrowf = T("p1_zrowf", [1, D], F32)
        Lb, h2b, Yb = K.Lb, K.h2b, K.Yb
        P.op("gpsimd", lambda e: e.memset(U[:], 1.0), writes=[Ub])
        P.op("gpsimd", lambda e: e.affine_select(out=U[:], in_=U[:], pattern=[[1, 128]], compare_op=ALU.is_gt, fill=0.0, base=0, channel_multiplier=-1), reads=[Ub], writes=[Ub])
        P.op("gpsimd", lambda e: e.iota(eoffi[:], pattern=[[CAP, 8]], base=0, channel_multiplier=0), writes=[eoffb])
        P.op("vector", lambda e: e.tensor_copy(out=eoff[:], in_=eoffi[:]), reads=[eoffb], writes=[eoffb])
        P.op("gpsimd", lambda e: e.iota(pidi[:], pattern=[[0, 1]], base=0, channel_multiplier=1), writes=[pidb])
        P.op("vector", lambda e: e.tensor_copy(out=pidf[:], in_=pidi[:]), reads=[pidb], writes=[pidb])
        P.op("vector", lambda e: e.memset(base[:], 0.0), writes=[baseb])
        P.op("gpsimd", lambda e: e.iota(cfill[:], pattern=[[0, NSLOT // 128]], base=NOWN, channel_multiplier=0), writes=[cfb])
        P.dma("sync", S["L"][0:NSLOT, :].rearrange("(p c) o -> p (c o)", p=128), cfill[:], reads=[cfb], writes=[Lb])
        P.op("vector", lambda e: e.memset(zrow[:], 0.0), writes=[zrb])
        P.op("vector", lambda e: e.memset(zrowf[:], 0.0), writes=[zrb])
        P.dma("sync", S["h2"][NOWN:NOWN + 1, :], zrow[:], reads=[zrb], writes=[h2b])
        for hf in range(2):
            P.dma("sync", S["Y"][hf][NSLOT:NSLOT + 1, :], zrowf[:], reads=[zrb], writes=[Yb])
        P.dma("gpsimd", wr[:], I["router"].rearrange("(k p) e -> p k e", p=128), writes=[wrb])
        cnt = {"w": 0, "ps": 0, "h": 0, "t": 0}

        def do_tile(t):
            q0 = t * 512
            P.dma("sync", att[:], S["att1"][:, :, q0:q0 + 512].rearrange("h p t -> p h t"), writes=[attb])
            P.dma("sync", xt[:], x1v[:, :, q0:q0 + 512], writes=[xb])
            for pc in range(4):
                w, wbuf = wt[cnt["w"] % 2], wtb[cnt["w"] % 2]
                cnt["w"] += 1
                P.dma("sync", w[:], wov[:, :, pc * 512:(pc + 1) * 512], reads=[K.Wb["w_out1"]], writes=[wbuf])
                for mm in range(4):
                    m = pc * 4 + mm
                    bi = cnt["ps"] % 4
                    cnt["ps"] += 1
                    for k in range(NK):
                        P.op("tensor", lambda e, k=k, mm=mm, w=w, bi=bi: e.matmul(K.ps[bi][:], w[:, k, mm * 128:(mm + 1) * 128], att[:, k, :], start=(k == 0), stop=(k == NK - 1)),
                             reads=[wbuf, attb], writes=[K.psb[bi]])
                    P.op("vector", lambda e, m=m, bi=bi: e.scalar_tensor_tensor(out=xt[:, m, :], in0=K.ps[bi][:], scalar=K.mod[:, l, 2, m, 0:1], in1=xt[:, m, :], op0=ALU.mult, op1=ALU.add),
                         reads=[K.psb[bi], xb, K.modb], writes=[xb])
            P.dma("sync", x2v[:, :, q0:q0 + 512], xt[:], reads=[xb], writes=[K.x2b])
            rms_modulate(K, xt, xb, ht, hb, 512, [(0, 512, 0)], l, 3, 4, tmp, tmpb, sq, sqb, rstd, rstdb)
            for s_ in range(4):
                if CUT == 11:
                    break
                sub = t * 4 + s_
                hk, hkb = htok[cnt["h"] % 2], htokb[cnt["h"] % 2]
                cnt["h"] += 1
                for q4 in range(4):
                    pw, pwb = K.pstw[q4 % 2], K.pstwb[q4 % 2]
                    for kk in range(4):
                        k = q4 * 4 + kk
                        P.op("tensor", lambda e, k=k, kk=kk, pw=pw, s_=s_: e.transpose(pw[:, kk * 128:(kk + 1) * 128], ht[:, k, s_ * 128:(s_ + 1) * 128], K.ident[:]), reads=[hb, K.cb], writes=[pwb])
                    if q4 % 2 == 0:
                        P.op("vector", lambda e, q4=q4, hk=hk, pw=pw: e.tensor_copy(out=hk[:, q4 * 512:(q4 + 1) * 512], in_=pw), reads=[pwb], writes=[hkb])
                    else:
                        P.op("vector", lambda e, q4=q4, hk=hk, pw=pw: e.tensor_copy(out=hk[:, q4 * 512:(q4 + 1) * 512], in_=pw), reads=[pwb], writes=[hkb])
                if not os.environ.get("KNOH2"):
                    P.dma("sync", S["h2"][q0 + s_ * 128:q0 + (s_ + 1) * 128, :], hk[:], reads=[hkb], writes=[h2b])
                if CUT == 12:
                    continue
                for k in range(NK):
                    P.op("tensor", lambda e, k=k, s_=s_: e.matmul(K.ps[4][:, 0:8], ht[:, k, s_ * 128:(s_ + 1) * 128], wr[:, k, :], start=(k == 0), stop=(k == NK - 1)), reads=[hb, wrb], writes=[K.psb[4]])
                P.op("vector", lambda e: e.tensor_copy(out=lg[:], in_=K.ps[4][:, 0:8]), reads=[K.psb[4]], writes=[lgb])
                P.op("vector", lambda e: e.max(out=mx[:], in_=lg[:]), reads=[lgb], writes=[mxb])
                for c in range(2):
                    P.op("vector", lambda e, c=c: e.tensor_scalar(out=ind[:, c, :], in0=lg[:], scalar1=mx[:, c:c + 1], scalar2=None, op0=ALU.is_equal), reads=[lgb, mxb], writes=[indb])
                P.op("vector", lambda e: e.tensor_tensor(out=indh[:], in0=ind[:, 0, :], in1=ind[:, 1, :], op=ALU.add), reads=[indb], writes=[indhb])
                P.op("vector", lambda e: e.tensor_tensor(out=dl[:], in0=mx[:, 0:1], in1=mx[:, 1:2], op=ALU.subtract), reads=[mxb], writes=[dlb])
                P.op("scalar", lambda e, sub=sub: e.activation(out=K.gate[:, sub, 0:1], in_=dl[:], func=AF.Sigmoid), reads=[dlb], writes=[K.gateb])
                P.op("vector", lambda e, sub=sub: e.tensor_scalar(out=K.gate[:, sub, 1:2], in0=K.gate[:, sub, 0:1], scalar1=-1.0, scalar2=1.0, op0=ALU.mult, op1=ALU.add), reads=[K.gateb], writes=[K.gateb])
                P.op("tensor", lambda e: e.matmul(K.ps[5][:, 0:8], U[:], indh[:], start=True, stop=True), reads=[Ub, indhb], writes=[K.psb[5]])
                P.op("tensor", lambda e: e.matmul(K.ps[5][:, 8:16], K.ones[:], indh[:], start=True, stop=True), reads=[K.cb, indhb], writes=[K.psb[5]])
                P.op("vector", lambda e: e.tensor_tensor(out=pos[:], in0=K.ps[5][:, 0:8], in1=base[:], op=ALU.add), reads=[K.psb[5], baseb], writes=[posb])
                P.op("vector", lambda e: e.tensor_tensor(out=base[:], in0=K.ps[5][:, 8:16], in1=base[:], op=ALU.add), reads=[K.psb[5], baseb], writes=[baseb])
                P.op("vector", lambda e: e.tensor_scalar(out=ind[:, 2, :], in0=pos[:], scalar1=float(CAP), scalar2=None, op0=ALU.is_lt), reads=[posb], writes=[indb])
                P.op("vector", lambda e: e.scalar_tensor_tensor(out=val[:], in0=pos[:], scalar=-float(NSLOT), in1=eoff[:], op0=ALU.add, op1=ALU.add), reads=[posb, eoffb], writes=[valb])
                P.op("vector", lambda e: e.tensor_tensor(out=val[:], in0=val[:], in1=ind[:, 2, :], op=ALU.mult), reads=[valb, indb], writes=[valb])
                P.op("vector", lambda e: e.tensor_scalar(out=val[:], in0=val[:], scalar1=float(NSLOT), scalar2=None, op0=ALU.add), reads=[valb], writes=[valb])
                for c in range(2):
                    P.op("vector", lambda e, c=c: e.tensor_tensor(out=junk8[:], in0=val[:], in1=ind[:, c, :], op=ALU.mult), reads=[valb, indb], writes=[junk8b])
                    P.op("vector", lambda e, c=c: e.tensor_reduce(out=slf[:, c:c + 1], in_=junk8[:], axis=AX.X, op=ALU.add), reads=[junk8b], writes=[slfb])
                P.op("vector", lambda e, sub=sub: e.tensor_copy(out=K.sloti[:, sub, :], in_=slf[:]), reads=[slfb], writes=[K.slotib])
                tk, tkb = toki[cnt["t"] % 2], tokb[cnt["t"] % 2]
                cnt["t"] += 1
                P.op("vector", lambda e, sub=sub: e.tensor_scalar(out=tokf[:], in0=pidf[:], scalar1=float(sub * 128), scalar2=None, op0=ALU.add), reads=[pidb], writes=[tkb])
                P.op("vector", lambda e, tk=tk: e.tensor_copy(out=tk[:], in_=tokf[:]), reads=[tkb], writes=[tkb])
                for c in range(2):
                    if CUT == 13:
                        continue
                    P._add("gpsimd", lambda e, c=c, sub=sub, tk=tk: e.indirect_dma_start(out=S["L"][:, :], out_offset=bass.IndirectOffsetOnAxis(ap=K.sloti[:, sub, c:c + 1], axis=0),
                                                                                        in_=tk[:], in_offset=None, bounds_check=NSLOT - 1, oob_is_err=False),
                           [K.slotib, tkb], [Lb], True)

        for t in range(4):
            if CUT == 10:
                break
            do_tile(t)
        P.barrier()


def stage_moe(K):
    nc, P, I, S = K.nc, K.P, K.I, K.S
    Lb, h2b, Yb = K.Lb, K.h2b, K.Yb
    with ExitStack() as st2:
        T = lambda n, s_, d: st2.enter_context(nc.sbuf_tensor(n, s_, d))
        hT = T("mo_hT", [128, NK, CAP], BF16); hTb = Buf()
        aT = T("mo_aT", [128, NJH, CAP], BF16); aTb = Buf()
        wt = [T(f"mo_w{i}", [128, NK, 512], BF16) for i in range(2)]; wtb = [Buf(), Buf()]
        wd = [T(f"mo_wd{i}", [128, NJH, 256], BF16) for i in range(2)]; wdb = [Buf(), Buf()]
        hg = [T(f"mo_hg{i}", [128, D], BF16) for i in range(2)]; hgb = [Buf(), Buf()]
        idx = [T(f"mo_idx{i}", [128, 1], I32) for i in range(2)]; idxb = [Buf(), Buf()]
        sl = [T(f"mo_sl{i}", [128, 512], F32) for i in range(2)]; slb = [Buf(), Buf()]
        ys = [T(f"mo_ys{i}", [128, 256], F32) for i in range(2)]; ysb = [Buf(), Buf()]
        cnt = {"w": 0, "wd": 0, "ps": 0, "g": 0, "sl": 0, "y": 0, "pst": 0}

        def expert(ex):
            wgv = I["exp_wg"][ex].rearrange("(k p) n -> p k n", p=128)
            wuv = I["exp_wu"][ex].rearrange("(k p) n -> p k n", p=128)
            wdv = I["exp_wd"][ex].rearrange("(j p) n -> p j n", p=128)
            for sb in range(CAP // 128):
                gi = cnt["g"] % 2
                cnt["g"] += 1
                r0 = ex * CAP + sb * 128
                P.dma("sync", idx[gi][:], S["L"][r0:r0 + 128, :], reads=[Lb], writes=[idxb[gi]])
                P._add("gpsimd", lambda e, gi=gi: e.indirect_dma_start(out=hg[gi][:], out_offset=None, in_=S["h2"][:, :],
                                                                       in_offset=bass.IndirectOffsetOnAxis(ap=idx[gi][:, 0:1], axis=0)),
                       [idxb[gi], h2b], [hgb[gi]], True)
                for q4 in range(4):
                    pw, pwb = K.pstw[q4 % 2], K.pstwb[q4 % 2]
                    for kk in range(4):
                        k = q4 * 4 + kk
                        P.op("tensor", lambda e, k=k, kk=kk, gi=gi, pw=pw: e.transpose(pw[:, kk * 128:(kk + 1) * 128], hg[gi][:, k * 128:(k + 1) * 128], K.ident[:]),
                             reads=[hgb[gi], K.cb], writes=[pwb])
                    if q4 % 2 == 0:
                        P.op("vector", lambda e, sb=sb, q4=q4, pw=pw: e.tensor_copy(out=hT[:, q4 * 4:(q4 + 1) * 4, sb * 128:(sb + 1) * 128], in_=pw.rearrange("p (k t) -> p k t", k=4)),
                             reads=[pwb], writes=[hTb])
                    else:
                        P.op("vector", lambda e, sb=sb, q4=q4, pw=pw: e.tensor_copy(out=hT[:, q4 * 4:(q4 + 1) * 4, sb * 128:(sb + 1) * 128], in_=pw.rearrange("p (k t) -> p k t", k=4)),
                             reads=[pwb], writes=[hTb])
            for hf in range(2):
                for jp in range(NJH // 2):
                    w, wbuf = wt[cnt["w"] % 2], wtb[cnt["w"] % 2]
                    cnt["w"] += 1
                    c0 = hf * (D_EXP // 2) + jp * 256
                    P.dma("gpsimd", w[:, :, 0:256], wgv[:, :, c0:c0 + 256], writes=[wbuf])
                    P.dma("gpsimd", w[:, :, 256:512], wuv[:, :, c0:c0 + 256], writes=[wbuf])
                    for jj in range(2):
                        j = jp * 2 + jj
                        for tt in range(CAP // 512):
                            bg = (cnt["ps"] % 2) * 2
                            cnt["ps"] += 1
                            for k in range(NK):
                                P.op("tensor", lambda e, k=k, jj=jj, w=w, bg=bg, tt=tt: e.matmul(K.ps[bg][:], w[:, k, jj * 128:(jj + 1) * 128], hT[:, k, tt * 512:(tt + 1) * 512], start=(k == 0), stop=(k == NK - 1)),
                                     reads=[wbuf, hTb], writes=[K.psb[bg]])
                            for k in range(NK):
                                P.op("tensor", lambda e, k=k, jj=jj, w=w, bg=bg, tt=tt: e.matmul(K.ps[bg + 1][:], w[:, k, 256 + jj * 128:256 + (jj + 1) * 128], hT[:, k, tt * 512:(tt + 1) * 512], start=(k == 0), stop=(k == NK - 1)),
                                     reads=[wbuf, hTb], writes=[K.psb[bg + 1]])
                            s_, s_b = sl[cnt["sl"] % 2], slb[cnt["sl"] % 2]
                            cnt["sl"] += 1
                            P.op("scalar", lambda e, bg=bg, s_=s_: e.activation(out=s_[:], in_=K.ps[bg][:], func=AF.Silu), reads=[K.psb[bg]], writes=[s_b])
                            P.op("vector", lambda e, bg=bg, s_=s_, j=j, tt=tt: e.tensor_tensor(out=aT[:, j, tt * 512:(tt + 1) * 512], in0=s_[:], in1=K.ps[bg + 1][:], op=ALU.mult),
                                 reads=[s_b, K.psb[bg + 1]], writes=[aTb])
                for ng in range(8):
                    w, wbuf = wd[cnt["wd"] % 2], wdb[cnt["wd"] % 2]
                    cnt["wd"] += 1
                    for jq in range(2):
                        P.dma("gpsimd", w[:, jq * 14:(jq + 1) * 14, :], wdv[:, hf * NJH + jq * 14:hf * NJH + (jq + 1) * 14, ng * 256:(ng + 1) * 256], writes=[wbuf])
                    for sb in range(CAP // 128):
                        bi = 4 + cnt["ps"] % 2
                        cnt["ps"] += 1
                        for j in range(NJH):
                            P.op("tensor", lambda e, j=j, sb=sb, w=w, bi=bi: e.matmul(K.ps[bi][:, 0:256], aT[:, j, sb * 128:(sb + 1) * 128], w[:, j, :], start=(j == 0), stop=(j == NJH - 1)),
                                 reads=[wbuf, aTb], writes=[K.psb[bi]])
                        y_, y_b = ys[cnt["y"] % 2], ysb[cnt["y"] % 2]
                        if cnt["y"] % 2 == 0:
                            P.op("vector", lambda e, y_=y_, bi=bi: e.tensor_copy(out=y_[:], in_=K.ps[bi][:, 0:256]), reads=[K.psb[bi]], writes=[y_b])
                        else:
                            P.op("scalar", lambda e, y_=y_, bi=bi: e.copy(out=y_[:], in_=K.ps[bi][:, 0:256]), reads=[K.psb[bi]], writes=[y_b])
                        cnt["y"] += 1
                        r0 = ex * CAP + sb * 128
                        P.dma("sync", S["Y"][hf][r0:r0 + 128, ng * 256:(ng + 1) * 256], y_[:], reads=[y_b], writes=[Yb])

        for ex in range(N_EXP):
            expert(ex)
        P.barrier()


def stage_final(K):
    nc, P, I, S = K.nc, K.P, K.I, K.S
    l = 1
    x2v = S["x2T"].rearrange("(k p) t -> p k t", p=128)
    outv = K.outT.rearrange("(k p) t -> p k t", p=128)
    finals = []
    with ExitStack() as st2:
        T = lambda n, s_, d: st2.enter_context(nc.sbuf_tensor(n, s_, d))
        yg = [T(f"fi_yg{i}", [128, D], F32) for i in range(4)]; ygb = [Buf() for _ in range(4)]
        acc = T("fi_acc", [128, D], F32); accb = Buf()
        xt = T("fi_x", [128, NK, 512], F32); xb = Buf()
        sq = T("fi_sq", [128, NK, 512], BF16); sqb = Buf()
        ot = T("fi_o", [128, NK, 512], F32); otb = Buf()
        tmp = [T(f"fi_tmp{i}", [128, 512], F32) for i in range(2)]; tmpb = [Buf(), Buf()]
        rstd = T("fi_rstd", [128, 512], F32); rstdb = Buf()
        identf = T("fi_identf", [128, 128], F32); idfb = Buf()
        gf = T("fi_gf", [128, 2, NK], F32); gfb = Buf()
        P.op("gpsimd", lambda e: e.memset(identf[:], 1.0), writes=[idfb])
        P.op("gpsimd", lambda e: e.affine_select(out=identf[:], in_=identf[:], pattern=[[-1, 128]], compare_op=ALU.is_equal, fill=0.0, base=0, channel_multiplier=1), reads=[idfb], writes=[idfb])
        P.op("vector", lambda e: e.memset(gf[:], 0.0), writes=[gfb])
        P.dma("sync", gf[:, 0, :], I["gfin"], writes=[gfb])
        for t in range(4):
            q0 = t * 512
            P.dma("sync", xt[:], x2v[:, :, q0:q0 + 512], reads=[K.x2b], writes=[xb])
            for s_ in range(4):
                sub = t * 4 + s_
                for c in range(2):
                    for hf in range(2):
                        i = c * 2 + hf
                        P._add("gpsimd", lambda e, i=i, c=c, hf=hf, sub=sub: e.indirect_dma_start(out=yg[i][:], out_offset=None, in_=S["Y"][hf][:, :],
                                                                                                in_offset=bass.IndirectOffsetOnAxis(ap=K.sloti[:, sub, c:c + 1], axis=0)),
                               [K.slotib, K.Yb], [ygb[i]], True)
                P.op("vector", lambda e: e.tensor_tensor(out=yg[0][:], in0=yg[0][:], in1=yg[1][:], op=ALU.add), reads=[ygb[0], ygb[1]], writes=[ygb[0]])
                P.op("gpsimd", lambda e: e.tensor_tensor(out=yg[2][:], in0=yg[2][:], in1=yg[3][:], op=ALU.add), reads=[ygb[2], ygb[3]], writes=[ygb[2]])
                P.op("vector", lambda e, sub=sub: e.tensor_scalar(out=acc[:], in0=yg[0][:], scalar1=K.gate[:, sub, 0:1], scalar2=None, op0=ALU.mult), reads=[ygb[0], K.gateb], writes=[accb])
                P.op("vector", lambda e, sub=sub: e.scalar_tensor_tensor(out=acc[:], in0=yg[2][:], scalar=K.gate[:, sub, 1:2], in1=acc[:], op0=ALU.mult, op1=ALU.add),
                     reads=[ygb[2], K.gateb, accb], writes=[accb])
                for grp in range(4):
                    bi = grp % 4
                    for mm in range(4):
                        m = grp * 4 + mm
                        P.op("tensor", lambda e, m=m, mm=mm, bi=bi: e.transpose(K.ps[bi][:, mm * 128:(mm + 1) * 128], acc[:, m * 128:(m + 1) * 128], identf[:]),
                             reads=[accb, idfb], writes=[K.psb[bi]])
                    for mm in range(4):
                        m = grp * 4 + mm
                        P.op("vector", lambda e, m=m, mm=mm, bi=bi, s_=s_: e.scalar_tensor_tensor(out=xt[:, m, s_ * 128:(s_ + 1) * 128], in0=K.ps[bi][:, mm * 128:(mm + 1) * 128],
                                                                                              scalar=K.mod[:, l, 5, m, 0:1], in1=xt[:, m, s_ * 128:(s_ + 1) * 128], op0=ALU.mult, op1=ALU.add),
                             reads=[K.psb[bi], xb, K.modb], writes=[xb])
            rms_modulate(K, xt, xb, ot, otb, 512, [(0, 512, 0)], l, 0, 0, tmp, tmpb, sq, sqb, rstd, rstdb,
                         A=lambda k, v: gf[:, 0, k:k + 1], B=lambda k, v: gf[:, 1, k:k + 1], extra_reads=[gfb])
            if K.debug:
                P.dma("sync", K.S["x3T"].rearrange("(k p) t -> p k t", p=128)[:, :, q0:q0 + 512], xt[:], reads=[xb])
            finals.append(P.dma("sync", outv[:, :, q0:q0 + 512], ot[:], reads=[otb]))
    return finals


def prep_core_inputs(inputs):
    x, c, ctx, c_ctx = inputs["x"], inputs["c"], inputs["ctx"], inputs["c_ctx"]
    shared = {}
    for l in (0, 1):
        shared[f"w_ada{l}"] = np.ascontiguousarray(inputs[f"l{l}_w_ada"], dtype=np.float32)
        shared[f"b_ada{l}"] = _fm(inputs[f"l{l}_b_ada"])
        shared[f"g1_{l}"] = _fm(inputs[f"l{l}_norm1_g"])
        shared[f"g2_{l}"] = _fm(inputs[f"l{l}_norm2_g"])
        shared[f"w_in{l}"] = np.ascontiguousarray(inputs[f"l{l}_w_in"], dtype=np.float32)
        shared[f"w_out{l}"] = np.ascontiguousarray(inputs[f"l{l}_w_out"], dtype=np.float32)
    shared["ffn_wg"] = np.ascontiguousarray(inputs["l0_ffn_w_gate"], dtype=np.float32)
    shared["ffn_wu"] = np.ascontiguousarray(inputs["l0_ffn_w_up"], dtype=np.float32)
    shared["ffn_wd"] = np.ascontiguousarray(inputs["l0_ffn_w_down"], dtype=np.float32)
    shared["router"] = np.ascontiguousarray(inputs["l1_router"], dtype=np.float32)
    shared["exp_wg"] = np.ascontiguousarray(inputs["l1_exp_w_gate"], dtype=np.float32)
    shared["exp_wu"] = np.ascontiguousarray(inputs["l1_exp_w_up"], dtype=np.float32)
    shared["exp_wd"] = np.ascontiguousarray(inputs["l1_exp_w_down"], dtype=np.float32)
    shared["gfin"] = _fm(inputs["final_norm_g"])
    shared["sinks"] = inputs["l1_sinks"].reshape(1, 8).astype(np.float32)
    shared["biasD"] = _bias_d(inputs["l1_rpb"].astype(np.float32))
    bias_e = [_bias_e(inputs["l1_rpb"].astype(np.float32), hf) for hf in range(2)]
    mask_c = [_mask_c(hf) for hf in range(2)]
    shared["lam"] = np.stack([inputs["l0_lam_q1"], inputs["l0_lam_k1"], inputs["l0_lam_q2"], inputs["l0_lam_k2"]])[None].astype(np.float32)
    shared["hvec"] = np.stack([inputs["l0_subln_g"], inputs["l0_q_norm_g"], inputs["l0_k_norm_g"]]).astype(np.float32)
    maps = []
    for ci in range(8):
        b, half = ci // 2, ci % 2
        order = core_token_order(half)
        m = dict(shared)
        xt = np.empty((D, NALL), np.float32)
        xt[:, :SEQ] = x[b][order].T
        xt[:, SEQ:] = ctx[b].T
        m["xT"] = xt
        cT = np.stack([_fm(c[b]), _fm(c_ctx)], axis=-1)
        m["cT"] = np.ascontiguousarray(cT, dtype=np.float32)
        m["biasE"] = bias_e[half]
        m["maskC"] = mask_c[half]
        m["rope"] = _rope_tables(np.concatenate([order, -np.ones(NCTX, np.int64)]))
        maps.append(m)
    return maps


_NC_CACHE = {}


def kernel(**inputs):
    inputs = {k: np.asarray(v) for k, v in inputs.items()}
    maps = prep_core_inputs(inputs)
    if "nc" not in _NC_CACHE:
        _NC_CACHE["nc"] = build_program()
    nc = _NC_CACHE["nc"]
    res = run_bass_kernel_spmd(nc, maps, core_ids=list(range(8)))
    out = np.empty((4, SEQ, D), np.float32)
    for ci in range(8):
        b, half = ci // 2, ci % 2
        out[b, half * NOWN:(half + 1) * NOWN] = res.results[ci]["outT"].T
    return out
```
